# Optimizing a Trainium2 kernel written in Bass

```python
import math
import jax, jax.numpy as jnp
from jax import lax
import numpy as np

D_MODEL = 1024
BATCH = 4
SEQ = 8192
DEPTH = 2

D_FF = 2816
EPS = 1e-6
N_EVEN = (DEPTH + 1) // 2
N_ODD = DEPTH // 2

GLA_HEADS = 4
GLA_VAL = D_MODEL // 2
GLA_DV = GLA_VAL // GLA_HEADS
GLA_DK = GLA_DV // 2
GLA_KEY = GLA_HEADS * GLA_DK
GLA_GATE_RANK = 16
GLA_TAU = 16.0
GLA_CHUNK = 64

POOL_WINDOWS = (2, 4, 8, 16)
POOL_GROUPS = 4
POOL_WIDTH = D_MODEL // 2
POOL_GDIM = POOL_WIDTH // POOL_GROUPS
IN0_WIDTH = 2 * GLA_KEY + 2 * GLA_VAL + GLA_GATE_RANK + POOL_WIDTH

NSA_HDIM = 64
NSA_HEADS = D_MODEL // NSA_HDIM
NSA_KV_GROUPS = 2
NSA_HPG = NSA_HEADS // NSA_KV_GROUPS
NSA_KV = NSA_KV_GROUPS * NSA_HDIM
CMP_BLOCK = 32
CMP_STRIDE = 16
CMP_HIDDEN = 256
SLC_BLOCK = 64
SLC_TOP = 16
WINDOW = 512
Q_BLOCK = 128
IN1_WIDTH = NSA_HEADS * NSA_HDIM + 6 * NSA_KV + 3 * NSA_HEADS
NEG = -1e30
FORCE = 1e4

kernel_name = "hybrid_gla_pool_nsa_macaron"


def rmsnorm(x, g):
    xf = x.astype(jnp.float32)
    y = xf * lax.rsqrt(jnp.mean(xf * xf, axis=-1, keepdims=True) + EPS)
    return (y * g.astype(jnp.float32)).astype(x.dtype)


def swiglu(x, wg, wu, wd):
    return (jax.nn.silu(x @ wg) * (x @ wu)) @ wd


def alibi_slopes(n):
    return np.array([2.0 ** (-8.0 * (i + 1) / n) for i in range(n)], dtype=np.float32)


def gla_chunked(q, k, v, log_a):
    B, T, H, dk = q.shape
    dv = v.shape[-1]
    C = GLA_CHUNK
    N = T // C

    def chunks(z):
        return z.reshape(B, N, C, H, z.shape[-1]).transpose(0, 3, 1, 2, 4)

    q, k, v, log_a = chunks(q * dk ** -0.5), chunks(k), chunks(v), chunks(log_a)
    b = jnp.cumsum(log_a, axis=3)
    b_last = b[:, :, :, -1:, :]
    q_dec = q * jnp.exp(b)
    k_inv = k * jnp.exp(-b)
    k_end = k * jnp.exp(b_last - b)
    causal = jnp.tril(jnp.ones((C, C), dtype=bool))
    att = jnp.where(causal, jnp.einsum('bhnid,bhnjd->bhnij', q_dec, k_inv), 0.0)
    o_intra = jnp.einsum('bhnij,bhnjv->bhniv', att, v)
    d_state = jnp.einsum('bhncd,bhncv->nbhdv', k_end, v)
    chunk_decay = jnp.exp(jnp.moveaxis(b_last[:, :, :, 0, :], 2, 0))

    def step(S, inp):
        dS, dec = inp
        return dec[..., None] * S + dS, S

    _, s_prev = lax.scan(step, jnp.zeros((B, H, dk, dv), q.dtype), (d_state, chunk_decay))
    o_inter = jnp.einsum('bhncd,nbhdv->bhncv', q_dec, s_prev)
    return (o_intra + o_inter).transpose(0, 2, 3, 1, 4).reshape(B, T, H, dv)


def pool_mixer(p, pool_w, pool_scale):
    B, T, _ = p.shape
    pf = p.astype(jnp.float32).reshape(B, T, POOL_GROUPS, POOL_GDIM)
    csum = jnp.cumsum(pf, axis=1)
    tpos = jnp.arange(T)
    groups = []
    for g, w in enumerate(POOL_WINDOWS):
        c = csum[:, :, g]
        c_lag = jnp.pad(c, ((0, 0), (w, 0), (0, 0)))[:, :T]
        cnt = jnp.minimum(tpos + 1, w).astype(jnp.float32)[None, :, None]
        groups.append((c - c_lag) / cnt - pf[:, :, g])
    pooled = jnp.stack(groups, axis=2)
    out = jnp.einsum('btgc,gcd->btgd', pooled, pool_w.astype(jnp.float32))
    return out.reshape(B, T, POOL_WIDTH) * pool_scale.astype(jnp.float32)


def mixer_gla_pool(h, w_in, gate_w2, gate_b, gla_norm, pool_w, pool_scale, w_out):
    B, T, _ = h.shape
    proj = h @ w_in
    cuts = np.cumsum([GLA_KEY, GLA_KEY, GLA_VAL, GLA_VAL, GLA_GATE_RANK]).tolist()
    q, k, v, g, gr, p = jnp.split(proj, cuts, axis=-1)
    f32 = jnp.float32
    log_a = jax.nn.log_sigmoid((gr @ gate_w2 + gate_b).astype(f32)) / GLA_TAU
    o = gla_chunked(q.astype(f32).reshape(B, T, GLA_HEADS, GLA_DK),
                    k.astype(f32).reshape(B, T, GLA_HEADS, GLA_DK),
                    v.astype(f32).reshape(B, T, GLA_HEADS, GLA_DV),
                    log_a.reshape(B, T, GLA_HEADS, GLA_DK))
    o = o * lax.rsqrt(jnp.mean(o * o, axis=-1, keepdims=True) + EPS) * gla_norm.astype(f32)
    o = o * jax.nn.silu(g.astype(f32).reshape(B, T, GLA_HEADS, GLA_DV))
    o_a = o.reshape(B, T, GLA_VAL)
    o_b = pool_mixer(p, pool_w, pool_scale)
    return jnp.concatenate([o_a, o_b], axis=-1).astype(h.dtype) @ w_out


def cmp_to_slc_matrix(ncmp, nslc):
    cs = np.arange(ncmp) * CMP_STRIDE
    ss = np.arange(nslc) * SLC_BLOCK
    ov = np.minimum(cs[:, None] + CMP_BLOCK, ss[None] + SLC_BLOCK) - np.maximum(cs[:, None], ss[None])
    return (np.clip(ov, 0, None) / CMP_BLOCK).astype(np.float32)


def nsa(h, w_in, cmp_pe, cmpk_w1, cmpk_w2, cmpv_w1, cmpv_w2, w_out):
    B, T, _ = h.shape
    G, HPG, dh = NSA_KV_GROUPS, NSA_HPG, NSA_HDIM
    f32 = jnp.float32
    proj = (h @ w_in).astype(f32)
    cuts = (NSA_HEADS * dh + NSA_KV * np.arange(7)).tolist()
    q, kc, vc, ks, vs, kw, vw, gates = jnp.split(proj, cuts, axis=-1)
    q = q.reshape(B, T, G, HPG, dh) * dh ** -0.5
    kc, vc, ks, vs, kw, vw = [z.reshape(B, T, G, dh) for z in (kc, vc, ks, vs, kw, vw)]
    gates = jax.nn.sigmoid(gates.reshape(B, T, G, HPG, 3))

    ncmp = (T - CMP_BLOCK) // CMP_STRIDE + 1
    cidx = np.arange(ncmp)[:, None] * CMP_STRIDE + np.arange(CMP_BLOCK)[None]
    cmp_end = jnp.asarray(cidx[:, -1])

    def compress(z, w1, w2):
        zb = z[:, cidx] + cmp_pe.astype(f32)[None, None, :, None, :]
        zb = zb.transpose(0, 1, 3, 2, 4).reshape(B, ncmp, G, CMP_BLOCK * dh)
        return jax.nn.gelu(zb @ w1) @ w2

    k_cmp = compress(kc, cmpk_w1, cmpk_w2).astype(f32)
    v_cmp = compress(vc, cmpv_w1, cmpv_w2).astype(f32)

    nslc = T // SLC_BLOCK
    n_top = min(SLC_TOP, nslc)
    m_cs = jnp.asarray(cmp_to_slc_matrix(ncmp, nslc))
    k_blk = ks.reshape(B, nslc, SLC_BLOCK, G, dh).transpose(0, 3, 1, 2, 4)
    v_blk = vs.reshape(B, nslc, SLC_BLOCK, G, dh).transpose(0, 3, 1, 2, 4)
    blk_ids = jnp.arange(nslc)
    gather = jax.vmap(jax.vmap(lambda kb, ix: kb[ix]))

    kw_pad = jnp.pad(kw, ((0, 0), (WINDOW, 0), (0, 0), (0, 0)))
    vw_pad = jnp.pad(vw, ((0, 0), (WINDOW, 0), (0, 0), (0, 0)))

    slopes = jnp.asarray(alibi_slopes(NSA_HEADS).reshape(HPG, G).T)

    def block(qb):
        t0 = qb * Q_BLOCK
        qblk = lax.dynamic_slice_in_dim(q, t0, Q_BLOCK, axis=1)
        gblk = lax.dynamic_slice_in_dim(gates, t0, Q_BLOCK, axis=1)
        tpos = t0 + jnp.arange(Q_BLOCK)

        dist_c = tpos[:, None] - cmp_end[None, :]
        valid_c = dist_c >= 0
        s_c = jnp.einsum('bqghd,bngd->bghqn', qblk, k_cmp) - slopes[:, :, None, None] * dist_c
        p_c = jax.nn.softmax(jnp.where(valid_c, s_c, NEG), axis=-1)
        p_c = jnp.where(valid_c.any(-1)[:, None], p_c, 0.0)
        o_c = jnp.einsum('bghqn,bngd->bqghd', p_c, v_cmp)

        imp = jnp.einsum('bghqn,nj->bgqj', p_c, m_cs)
        cur = tpos // SLC_BLOCK
        forced = (blk_ids[None] == 0) | (blk_ids[None] == cur[:, None]) | (blk_ids[None] == cur[:, None] - 1)
        valid_b = (blk_ids[None] * SLC_BLOCK) <= tpos[:, None]
        imp = jnp.where(valid_b, jnp.where(forced, FORCE, imp), -1.0)
        _, sel = lax.top_k(imp, n_top)
        k_sel = gather(k_blk, sel)
        v_sel = gather(v_blk, sel)
        spos = sel[..., None] * SLC_BLOCK + jnp.arange(SLC_BLOCK)
        dist_s = (tpos[None, None, :, None, None] - spos)[:, :, None]
        s_s = jnp.einsum('bqghd,bgqnld->bghqnl', qblk, k_sel) - slopes[None, :, :, None, None, None] * dist_s
        s_s = jnp.where(dist_s >= 0, s_s, NEG)
        sh = s_s.shape
        p_s = jax.nn.softmax(s_s.reshape(sh[:4] + (-1,)), axis=-1).reshape(sh)
        o_s = jnp.einsum('bghqnl,bgqnld->bqghd', p_s, v_sel)

        k_win = lax.dynamic_slice_in_dim(kw_pad, t0, Q_BLOCK + WINDOW, axis=1)
        v_win = lax.dynamic_slice_in_dim(vw_pad, t0, Q_BLOCK + WINDOW, axis=1)
        wpos = t0 - WINDOW + jnp.arange(Q_BLOCK + WINDOW)
        dist_w = tpos[:, None] - wpos[None, :]
        valid_w = (dist_w >= 0) & (dist_w < WINDOW) & (wpos[None, :] >= 0)
        s_w = jnp.einsum('bqghd,bkgd->bghqk', qblk, k_win) - slopes[:, :, None, None] * dist_w
        p_w = jax.nn.softmax(jnp.where(valid_w, s_w, NEG), axis=-1)
        o_w = jnp.einsum('bghqk,bkgd->bqghd', p_w, v_win)

        return gblk[..., 0:1] * o_c + gblk[..., 1:2] * o_s + gblk[..., 2:3] * o_w

    outs = lax.map(block, jnp.arange(T // Q_BLOCK))
    o = outs.transpose(1, 0, 2, 3, 4, 5).reshape(B, T, NSA_HEADS * dh)
    return o.astype(h.dtype) @ w_out


def setup_inputs(seed: int = 0) -> dict:
    key = jax.random.key(seed)
    ks = iter(jax.random.split(key, 40))
    nrm = lambda shape, scale: jax.random.normal(next(ks), shape, jnp.float32) * scale
    gain = lambda shape: 1.0 + nrm(shape, 0.02)
    D, F = D_MODEL, D_FF
    CIN = CMP_BLOCK * NSA_HDIM
    return {
        "x": nrm((BATCH, SEQ, D), 1.0),
        "norm_ffn1": gain((DEPTH, D)),
        "ffn1_wg": nrm((DEPTH, D, F), D ** -0.5),
        "ffn1_wu": nrm((DEPTH, D, F), D ** -0.5),
        "ffn1_wd": nrm((DEPTH, F, D), F ** -0.5),
        "norm_mix": gain((DEPTH, D)),
        "norm_ffn2": gain((DEPTH, D)),
        "ffn2_wg": nrm((DEPTH, D, F), D ** -0.5),
        "ffn2_wu": nrm((DEPTH, D, F), D ** -0.5),
        "ffn2_wd": nrm((DEPTH, F, D), F ** -0.5),
        "a_w_in": nrm((N_EVEN, D, IN0_WIDTH), D ** -0.5),
        "a_gate_w2": nrm((N_EVEN, GLA_GATE_RANK, GLA_KEY), GLA_GATE_RANK ** -0.5),
        "a_gate_b": nrm((N_EVEN, GLA_KEY), 0.1),
        "a_gla_norm": gain((N_EVEN, GLA_DV)),
        "a_pool_w": nrm((N_EVEN, POOL_GROUPS, POOL_GDIM, POOL_GDIM), POOL_GDIM ** -0.5),
        "a_pool_scale": gain((N_EVEN, POOL_WIDTH)),
        "a_w_out": nrm((N_EVEN, D, D), D ** -0.5),
        "c_w_in": nrm((N_ODD, D, IN1_WIDTH), D ** -0.5),
        "c_cmp_pe": nrm((N_ODD, CMP_BLOCK, NSA_HDIM), 0.1),
        "c_cmpk_w1": nrm((N_ODD, CIN, CMP_HIDDEN), CIN ** -0.5),
        "c_cmpk_w2": nrm((N_ODD, CMP_HIDDEN, NSA_HDIM), CMP_HIDDEN ** -0.5),
        "c_cmpv_w1": nrm((N_ODD, CIN, CMP_HIDDEN), CIN ** -0.5),
        "c_cmpv_w2": nrm((N_ODD, CMP_HIDDEN, NSA_HDIM), CMP_HIDDEN ** -0.5),
        "c_w_out": nrm((N_ODD, D, D), D ** -0.5),
        "final_norm": gain((D,)),
    }


def reference(x, norm_ffn1, ffn1_wg, ffn1_wu, ffn1_wd, norm_mix, norm_ffn2, ffn2_wg, ffn2_wu, ffn2_wd,
              a_w_in, a_gate_w2, a_gate_b, a_gla_norm, a_pool_w, a_pool_scale, a_w_out,
              c_w_in, c_cmp_pe, c_cmpk_w1, c_cmpk_w2, c_cmpv_w1, c_cmpv_w2, c_w_out, final_norm):
    h = x
    for l in range(DEPTH):
        h = h + 0.5 * swiglu(rmsnorm(h, norm_ffn1[l]), ffn1_wg[l], ffn1_wu[l], ffn1_wd[l])
        hn = rmsnorm(h, norm_mix[l])
        i = l // 2
        if l % 2 == 0:
            mix = mixer_gla_pool(hn, a_w_in[i], a_gate_w2[i], a_gate_b[i], a_gla_norm[i],
                                 a_pool_w[i], a_pool_scale[i], a_w_out[i])
        else:
            mix = nsa(hn, c_w_in[i], c_cmp_pe[i], c_cmpk_w1[i], c_cmpk_w2[i],
                      c_cmpv_w1[i], c_cmpv_w2[i], c_w_out[i])
        h = h + mix.astype(h.dtype)
        h = h + 0.5 * swiglu(rmsnorm(h, norm_ffn2[l]), ffn2_wg[l], ffn2_wu[l], ffn2_wd[l])
    return rmsnorm(h, final_norm)
```

```python
import contextlib
import math
import numpy as np
import ml_dtypes
import concourse.bass as bass
import concourse.mybir as mybir
from concourse.bass_utils import run_bass_kernel_spmd

F32 = mybir.dt.float32
BF16 = mybir.dt.bfloat16
AF = mybir.ActivationFunctionType
ALU = mybir.AluOpType
SEM_LIMIT = 30000

D = 1024
DFF = 2816
NF = DFF // 128
TT = 512
EPS = 1e-6
IN0 = 2064
IN1 = 1840
BIG = 30000.0
NHEAD = 16
SLOPES = [2.0 ** (-8.0 * (i + 1) / 16) for i in range(16)]
def head_slope(h):
    g, hh = divmod(h, 8)
    return SLOPES[hh * 2 + g]


class Tk:
    __slots__ = ("ap", "lw", "rd", "name")

    def __init__(self, ap, name=""):
        self.ap = ap
        self.lw = None
        self.rd = {}
        self.name = name

    def __getitem__(self, idx):
        return self.ap[idx]


class _Rec:
    def __init__(self):
        self.call = None

    def __getattr__(self, name):
        def f(*a, **k):
            self.call = (name, a, k)
            return self
        return f


class Eng:
    def __init__(self, ctx, name, is_pe=False):
        self.ctx = ctx
        self.name = name
        self.is_pe = is_pe
        self.sem = ctx.nc.alloc_semaphore(name + "_s0")
        self.epoch = 0
        self.count = 0
        self.waited = {}
        self.prog = []

    def _wait(self, tok):
        if tok is None:
            return
        sem, val, eng = tok
        if eng is self and self.is_pe:
            return
        k = id(sem)
        if self.waited.get(k, 0) >= val:
            return
        self.prog.append(lambda e, sem=sem, val=val: e.wait_ge(sem, val))
        self.waited[k] = val

    def deps(self, reads, writes):
        for t in reads:
            self._wait(t.lw)
        for t in writes:
            self._wait(t.lw)
            for tok in t.rd.values():
                self._wait(tok)

    def op(self, build, reads=(), writes=()):
        self.deps(reads, writes)
        if self.count >= SEM_LIMIT:
            self.epoch += 1
            self.sem = self.ctx.nc.alloc_semaphore("%s_s%d" % (self.name, self.epoch))
            self.count = 0
        self.count += 1
        r = _Rec()
        build(r)
        nm, a, k = r.call
        self.prog.append(lambda e, nm=nm, a=a, k=k, sem=self.sem: getattr(e, nm)(*a, **k).then_inc(sem, 1))
        tok = (self.sem, self.count, self)
        for t in reads:
            t.rd[self.name] = tok
        for t in writes:
            t.lw = tok
            t.rd = {}
        return tok


class DSem:
    def __init__(self, ctx, name):
        self.sem = ctx.nc.alloc_semaphore(name)
        self.count = 0
        self.name = name


class Ctx:
    def __init__(self, nc):
        self.nc = nc
        self.pe = Eng(self, "pe", is_pe=True)
        self.act = Eng(self, "act")
        self.dve = Eng(self, "dve")
        self.pool = Eng(self, "pool")
        self.sp = Eng(self, "sp")
        self.engs = [self.pe, self.act, self.dve, self.pool, self.sp]
        self.dsems = []

    def dsem(self, name=None):
        d = DSem(self, "%s_%d" % (name or "ds", len(self.dsems)))
        self.dsems.append(d)
        return d

    def dma(self, q, ds, out, in_, reads=(), writes=(), n=1, **kw):
        q.deps(reads, writes)
        pairs = list(zip(out, in_)) if isinstance(out, (list, tuple)) else [(out, in_)]
        for o, i in pairs:
            q.prog.append(lambda e, o=o, i=i, sem=ds.sem, kw=kw: e.dma_start(out=o, in_=i, **kw).then_inc(sem, 16))
            ds.count += 16
        tok = (ds.sem, ds.count, None)
        for t in reads:
            t.rd["dma_" + ds.name] = tok
        for t in writes:
            t.lw = tok
            t.rd = {}
        return tok

    def barrier(self):
        toks = [(E.sem, E.count, E) for E in self.engs if E.count > 0]
        toks += [(d.sem, d.count, None) for d in self.dsems if d.count > 0]
        for E in self.engs:
            for tok in toks:
                if tok[2] is E:
                    continue
                E._wait(tok)

    def emit(self):
        self.barrier()
        with self.nc.Block() as blk:
            def mk(E):
                def body(e):
                    for f in E.prog:
                        f(e)
                return body
            blk.tensor(mk(self.pe))
            blk.scalar(mk(self.act))
            blk.vector(mk(self.dve))
            blk.gpsimd(mk(self.pool))
            blk.sync(mk(self.sp))


class Scope:
    def __init__(self, c):
        self.c = c
        self.st = contextlib.ExitStack()

    _uid = [0]

    def sb(self, name, shape, dtype):
        Scope._uid[0] += 1
        t = self.st.enter_context(self.c.nc.sbuf_tensor("sb%d_%s" % (Scope._uid[0], name), list(shape), dtype))
        return Tk(t.ap() if hasattr(t, "ap") and callable(t.ap) else t, name)

    def close(self):
        self.c.barrier()
        self.st.close()


class Pipe:
    def __init__(self, depth=2):
        self.q = []
        self.depth = depth

    def push(self, fn):
        self.q.append(fn)
        while len(self.q) > self.depth:
            self.q.pop(0)()

    def flush(self):
        while self.q:
            self.q.pop(0)()


class WStream:
    def __init__(self, c, sc, nslots, width):
        self.c = c
        self.slots = [sc.sb("wslot%d" % i, [128, width], BF16) for i in range(nslots)]
        self.ds = [c.dsem("wsd%d" % i) for i in range(nslots)]
        self.queue = []
        self.inflight = []
        self.k = 0

    def push(self, items):
        self.queue.extend(items)
        self._fill()

    def _fill(self):
        while self.queue and len(self.inflight) < len(self.slots):
            it = self.queue.pop(0)
            i = self.k % len(self.slots)
            self.k += 1
            slot = self.slots[i]
            outs = [f(slot.ap) for f, _ in it]
            ins = [s for _, s in it]
            self.c.dma(self.c.pool, self.ds[i], outs, ins, writes=[slot], n=len(it))
            self.inflight.append(slot)

    def next(self):
        s = self.inflight.pop(0)
        return s

    def done(self):
        self._fill()


def host_consts(T=8192):
    cst = {}
    j = np.arange(128)[:, None]
    i = np.arange(128)[None, :]
    same = (j // 64) == (i // 64)
    cst["ltri"] = (np.where(same & (j <= i), -1.0 / 16, 0.0)).astype(np.float32)
    cst["mgt"] = (np.where(same & (j > i), -1.0 / 16, 0.0)).astype(np.float32)
    cst["mask01"] = (np.where(same & (j <= i), 1.0, 0.0)).astype(np.float32)
    cst["invc"] = np.tile((1.0 / (np.arange(16) + 1.0))[None, :], (128, 1)).astype(np.float32)
    cb = np.zeros((128, 256), np.float32)
    for p in range(128):
        cur = p // 64
        for rel in range(-128, 128):
            if rel > cur:
                v = -1.0
            elif rel == cur or rel == cur - 1:
                v = 1e4
            else:
                v = 0.0
            cb[p, 128 + rel] = v
    cst["cb"] = cb
    ew = np.zeros((128, 32, 128), np.float32)
    for p in range(128):
        r = p % 64
        for pt in range(32):
            for half in range(2):
                if r == 2 * pt + half:
                    ew[p, pt, half * 64:(half + 1) * 64] = 1.0
    cst["ew"] = ew.astype(ml_dtypes.bfloat16)
    k = np.arange(128)[:, None]
    q = np.arange(512)[None, :]
    cm = np.zeros((128, 4, 512), np.float32)
    wm = np.zeros((128, 4, 512), np.float32)
    for jj in range(4):
        cm[:, jj, :] = np.where(q < 128 * jj + k, -BIG, 0.0)
        wm[:, jj, :] = np.where(q >= 128 * jj + k, -BIG, 0.0)
    cst["cmask"] = cm.astype(ml_dtypes.bfloat16)
    cst["wmask"] = wm.astype(ml_dtypes.bfloat16)
    pm = np.zeros((128, 5, 512), np.float32)
    for v in range(5):
        off = 512 * v
        pm[:, v, :] = np.where(16 * k + 31 <= off + q, 0.0, -BIG)
    cst["pmask"] = pm.astype(ml_dtypes.bfloat16)
    qa = np.zeros((3, 16, 512), np.float32)
    for h in range(16):
        s = head_slope(h)
        s_hi = np.float32(np.float32(s).astype(ml_dtypes.bfloat16))
        s_lo = np.float32(s) - s_hi
        qa[0, h, :] = -s * np.arange(512)
        qa[1, h, :] = s_hi
        qa[2, h, :] = s_lo
    cst["qaug"] = qa.astype(ml_dtypes.bfloat16)
    ka = np.zeros((3, 128), np.float32)
    ka[0] = 1.0
    ka[1] = np.arange(128)
    ka[2] = np.arange(128)
    cst["kaug"] = ka.astype(ml_dtypes.bfloat16)
    kc = ka.copy()
    kc[1] *= 16
    kc[2] *= 16
    cst["kcaug"] = kc.astype(ml_dtypes.bfloat16)
    NCK = max(1, T // 2048)
    cst["kaugT"] = np.tile(ka[:, None, :], (1, 2, T // 128)).reshape(3, 2, T).astype(ml_dtypes.bfloat16)
    cst["kcaugT"] = np.tile(kc[:, None, :], (1, 2, NCK)).reshape(3, 2, NCK * 128).astype(ml_dtypes.bfloat16)
    n = np.arange(512)
    cs = n * 16
    ss = np.arange(128) * 64
    ov = np.minimum(cs[:, None] + 32, ss[None] + 64) - np.maximum(cs[:, None], ss[None])
    mcs = (np.clip(ov, 0, None) / 32.0).astype(np.float32)
    mcs[511] = 0.0
    cst["mcs"] = mcs.reshape(4, 128, 128).transpose(1, 0, 2).copy().astype(ml_dtypes.bfloat16)
    ident = np.eye(128, dtype=np.float32)
    cst["identf"] = ident
    cst["identb"] = ident.astype(ml_dtypes.bfloat16)
    cst["onesb"] = np.full((128, 128), 1.0 / D, np.float32).astype(ml_dtypes.bfloat16)
    return cst


CONST_DT = {"ltri": F32, "mgt": F32, "mask01": F32, "invc": F32, "cb": F32, "ew": BF16, "cmask": BF16,
            "wmask": BF16, "pmask": BF16, "qaug": BF16, "kaug": BF16, "kcaug": BF16, "kaugT": BF16, "kcaugT": BF16, "mcs": BF16,
            "identf": F32, "identb": BF16, "onesb": BF16}

IN_SHAPES = {
    "ffn_wg": [4, D, DFF], "ffn_wu": [4, D, DFF], "ffn_wd": [4, DFF, D],
    "gam": [128, 7 * 8], "a_w_in": [D, IN0], "w2a": [33, 256], "gnorm": [1, 128], "poolw": [4, 128, 128],
    "pscale": [128, 4], "a_w_out": [D, D], "c_w_in": [D, IN1], "pef": [128, 16],
    "cmpk_w1": [2048, 256], "cmpk_w2": [256, 64], "cmpv_w1": [2048, 256], "cmpv_w2": [256, 64], "c_w_out": [D, D],
}


def bias_plan(T):
    NT = T // TT
    plan = {}
    for h in range(16):
        for d in range(-3, 4 * NT + 1):
            plan[("s", h, d)] = len(plan)
    for h in range(16):
        for qt in range(NT):
            t0 = qt * TT
            nkc = ((t0 + 511 - 31) // 16) // 128 + 1
            for ktc in range(nkc):
                key = ("c", h, t0 - 2048 * ktc - 31)
                if key not in plan:
                    plan[key] = len(plan)
    return plan


def bias_table(T):
    plan = bias_plan(T)
    tab = np.zeros((128, len(plan)), np.float32)
    for (kind, h, v), i in plan.items():
        s_ = head_slope(h)
        tab[:, i] = -s_ * (128.0 * v if kind == "s" else float(v))
    return tab

def build_nc(T, stage="full"):
    NT = T // TT
    nc = bass.Bass("TRN2", target_bir_lowering=False)
    cst = host_consts(T)
    A = {}
    A["x"] = nc.dram_tensor("x", [T, D], F32, kind="ExternalInput").ap()
    for k, shp in IN_SHAPES.items():
        A[k] = nc.dram_tensor(k, shp, F32, kind="ExternalInput").ap()
    for k, v in cst.items():
        A[k] = nc.dram_tensor("c_" + k, list(v.shape), CONST_DT[k], kind="ExternalInput").ap()
    A["biasT"] = nc.dram_tensor("biasT", [128, len(bias_plan(T))], F32, kind="ExternalInput").ap()
    out = nc.dram_tensor("out", [T, D], F32, kind="ExternalOutput").ap()
    HS = Tk(nc.dram_tensor("hs", [128, 8, T], F32, kind="Internal").ap(), "hs")
    QS = Tk(nc.dram_tensor("qs", [64, 16, T], BF16, kind="Internal").ap(), "qs")
    KSs = {nm: Tk(nc.dram_tensor("s_" + nm, [64, 2, T], BF16, kind="Internal").ap(), nm) for nm in ("ks", "kw", "kc", "vc")}
    VSs = {nm: Tk(nc.dram_tensor("s_" + nm, [T, 2, 64], BF16, kind="Internal").ap(), nm) for nm in ("vs", "vw")}
    GS = Tk(nc.dram_tensor("gs", [T, 48], F32, kind="Internal").ap(), "gs")
    OS = Tk(nc.dram_tensor("os", [128, 8, T], BF16, kind="Internal").ap(), "os")

    c = Ctx(nc)
    P = [Tk(nc.alloc_psum_tensor("ps%d" % i, [128, 512], F32).ap(), "ps%d" % i) for i in range(8)]

    g_sc = Scope(c)
    onesb = g_sc.sb("onesb", [128, 128], BF16)
    identf = g_sc.sb("identf", [128, 128], F32)
    identb = g_sc.sb("identb", [128, 128], BF16)
    gam = g_sc.sb("gam", [128, 56], F32)
    epsb = g_sc.sb("epsb", [128, 1], F32)
    oneb = g_sc.sb("oneb", [128, 1], F32)
    c.dve.op(lambda e: e.memset(epsb[:], EPS), writes=[epsb])
    c.dve.op(lambda e: e.memset(oneb[:], 1.0), writes=[oneb])
    dsc = c.dsem("dconst")
    c.dma(c.sp, dsc, [onesb[:], identf[:], identb[:], gam[:]], [A["onesb"], A["identf"], A["identb"], A["gam"]],
          writes=[onesb, identf, identb, gam], n=4)

    def rmsnorm_fm(sc_bufs, hT, gi, hn):
        sq, lnb, rstd = sc_bufs
        for cc in range(8):
            s = sq[cc % 2]
            c.act.op(lambda e, cc=cc, s=s: e.activation(out=s[:], in_=hT[:, cc, :], func=AF.Square), reads=[hT], writes=[s])
            c.pe.op(lambda e, cc=cc, s=s: e.matmul(P[6][:, :], lhsT=onesb[:], rhs=s[:], start=(cc == 0), stop=(cc == 7)),
                    reads=[s, onesb], writes=[P[6]])
        c.act.op(lambda e: e.activation(out=lnb[:], in_=P[6][:, :], func=AF.Ln, bias=epsb[:], scale=1.0), reads=[P[6], epsb], writes=[lnb])
        c.act.op(lambda e: e.activation(out=rstd[:], in_=lnb[:], func=AF.Exp, scale=-0.5), reads=[lnb], writes=[rstd])
        for cc in range(8):
            c.dve.op(lambda e, cc=cc: e.scalar_tensor_tensor(out=hn[:, cc, :], in0=hT[:, cc, :], scalar=gam[:, gi * 8 + cc:gi * 8 + cc + 1],
                                                             op0=ALU.mult, in1=rstd[:], op1=ALU.mult),
                     reads=[hT, rstd, gam], writes=[hn])

    def ffn_items(fi):
        items = []
        for j in range(NF):
            items.append([
                (lambda s: s[:, 0:1024].rearrange("p (c f) -> p c f", c=8), A["ffn_wg"][fi, :, j * 128:(j + 1) * 128].rearrange("(c p) f -> p c f", p=128)),
                (lambda s: s[:, 1024:2048].rearrange("p (c f) -> p c f", c=8), A["ffn_wu"][fi, :, j * 128:(j + 1) * 128].rearrange("(c p) f -> p c f", p=128)),
            ])
        for cc in range(8):
            items.append([
                (lambda s: s[:, 0:2816].rearrange("p (j f) -> p j f", j=NF), A["ffn_wd"][fi, :, cc * 128:(cc + 1) * 128].rearrange("(j p) f -> p j f", p=128)),
            ])
        return items

    def ffn(ws, bufs, hT, gi):
        hn, aT, sgs, nb = bufs
        rmsnorm_fm(nb, hT, gi, hn)
        for j in range(NF):
            w = ws.next()
            pg, pu = P[j % 2], P[2 + j % 2]
            for cc in range(8):
                c.pe.op(lambda e, cc=cc, w=w, pg=pg: e.matmul(pg[:, :], lhsT=w[:, cc * 128:(cc + 1) * 128], rhs=hn[:, cc, :], start=(cc == 0), stop=(cc == 7)),
                        reads=[w, hn], writes=[pg])
            for cc in range(8):
                c.pe.op(lambda e, cc=cc, w=w, pu=pu: e.matmul(pu[:, :], lhsT=w[:, 1024 + cc * 128:1024 + (cc + 1) * 128], rhs=hn[:, cc, :], start=(cc == 0), stop=(cc == 7)),
                        reads=[w, hn], writes=[pu])
            ws.done()
            sg = sgs[j % 2]
            c.act.op(lambda e, pg=pg, sg=sg: e.activation(out=sg[:], in_=pg[:, :], func=AF.Silu), reads=[pg], writes=[sg])
            c.dve.op(lambda e, j=j, pu=pu, sg=sg: e.tensor_tensor(out=aT[:, j, :], in0=pu[:, :], in1=sg[:], op=ALU.mult), reads=[pu, sg], writes=[aT])
        for cc in range(8):
            w = ws.next()
            py = P[4 + cc % 2]
            for j in range(NF):
                c.pe.op(lambda e, j=j, w=w, py=py: e.matmul(py[:, :], lhsT=w[:, j * 128:(j + 1) * 128], rhs=aT[:, j, :], start=(j == 0), stop=(j == NF - 1)),
                        reads=[w, aT], writes=[py])
            ws.done()
            c.dve.op(lambda e, cc=cc, py=py: e.scalar_tensor_tensor(out=hT[:, cc, :], in0=py[:, :], scalar=0.5, op0=ALU.mult, in1=hT[:, cc, :], op1=ALU.add),
                     reads=[py, hT], writes=[hT])

    def load_resident(sc, name, src, ncols, ds=None):
        ds = c.dsem("d_" + name)
        t = sc.sb(name, [128, 8, ncols], BF16)
        step = 512
        outs, ins = [], []
        for c0 in range(0, ncols, step):
            c1 = min(ncols, c0 + step)
            outs.append(t[:, :, c0:c1])
            ins.append(src[:, c0:c1].rearrange("(c p) f -> p c f", p=128))
        c.dma(c.pool, ds, outs, ins, writes=[t], n=len(outs))
        return t

    def ffn_bufs(sc):
        hn = sc.sb("hn", [128, 8, TT], BF16)
        aT = sc.sb("aT", [128, NF, TT], BF16)
        sgs = [sc.sb("sg%d" % i, [128, TT], F32) for i in range(2)]
        sq = [sc.sb("sq%d" % i, [128, TT], BF16) for i in range(2)]
        lnb = sc.sb("lnb", [128, TT], F32)
        rstd = sc.sb("rstd", [128, TT], F32)
        return (hn, aT, sgs, (sq, lnb, rstd))

    def load_x_tile(t, xt, hT, ds):
        c.dma(c.sp, ds, xt[:], A["x"][t * TT:(t + 1) * TT, :].rearrange("(s p) d -> p s d", p=128), writes=[xt])
        for cc in range(8):
            pb = P[cc % 2]
            for s in range(4):
                c.pe.op(lambda e, cc=cc, s=s, pb=pb: e.transpose(out=pb[:, s * 128:(s + 1) * 128], in_=xt[:, s, cc * 128:(cc + 1) * 128], identity=identf[:]),
                        reads=[xt, identf], writes=[pb])
            c.act.op(lambda e, cc=cc, pb=pb: e.copy(out=hT[:, cc, :], in_=pb[:, :]), reads=[pb], writes=[hT])

    def store_out_tile(t, src, xt, ds):
        for s in range(4):
            for half in range(2):
                pb = P[(2 * s + half) % 2]
                for k in range(4):
                    cc = half * 4 + k
                    c.pe.op(lambda e, cc=cc, s=s, k=k, pb=pb: e.transpose(out=pb[:, k * 128:(k + 1) * 128], in_=src[:, cc, s * 128:(s + 1) * 128], identity=identf[:]),
                            reads=[src, identf], writes=[pb])
                c.act.op(lambda e, s=s, half=half, pb=pb: e.copy(out=xt[:, s, half * 512:(half + 1) * 512], in_=pb[:, :]), reads=[pb], writes=[xt])
        return c.dma(c.sp, ds, out[t * TT:(t + 1) * TT, :].rearrange("(s p) d -> p s d", p=128), xt[:], reads=[xt])

    sc = Scope(c)
    ws = WStream(c, sc, 3, 2816)
    fb = ffn_bufs(sc)
    hn = fb[0]
    hT = sc.sb("hT", [128, 8, TT], F32)
    xt = sc.sb("xt", [128, 4, D], F32)
    dres = c.dsem("dres")
    win0 = load_resident(sc, "win0", A["a_w_in"], IN0, dres)
    wout0 = load_resident(sc, "wout0", A["a_w_out"], D, dres)
    dx = c.dsem("dx")
    dhs = c.dsem("dhs")
    ltri = sc.sb("ltri", [128, 128], F32)
    mgt = sc.sb("mgt", [128, 128], F32)
    mask01 = sc.sb("mask01", [128, 128], F32)
    invc = sc.sb("invc", [128, 16], F32)
    w2a = sc.sb("w2a", [33, 256], F32)
    gnb = sc.sb("gnb", [128, 128], F32)
    poolw = sc.sb("poolw", [128, 4, 128], BF16)
    pscale = sc.sb("pscale", [128, 4], F32)
    dca = c.dsem("dca")
    c.dma(c.sp, dca, [ltri[:], mgt[:], mask01[:], invc[:], w2a[:], gnb[:], pscale[:]],
          [A["ltri"], A["mgt"], A["mask01"], A["invc"], A["w2a"], A["gnorm"].partition_broadcast(128), A["pscale"]],
          writes=[ltri, mgt, mask01, invc, w2a, gnb, pscale], n=7)
    c.dma(c.pool, c.dsem("dpoolw"), poolw[:], A["poolw"].rearrange("g c d -> c g d"), writes=[poolw])
    qT = sc.sb("qT", [128, 2, TT], F32)
    kT = sc.sb("kT", [128, 2, TT], F32)
    gra = sc.sb("gra", [33, TT], F32)
    c.dve.op(lambda e: e.memset(gra[:], 0.0), writes=[gra])
    c.dve.op(lambda e: e.memset(gra[32:33, :], 1.0), writes=[gra])
    lsp = sc.sb("lsp", [128, 256], F32)
    ebT = sc.sb("ebT", [128, 2, 128], F32)
    enbT = sc.sb("enbT", [128, 2, 128], F32)
    ekend = sc.sb("ekend", [128, 256], F32)
    qdec = sc.sb("qdec", [128, 2, 128], BF16)
    kinv = sc.sb("kinv", [128, 2, 128], BF16)
    kend = sc.sb("kend", [128, 256], BF16)
    vtok = sc.sb("vtok", [128, 512], BF16)
    sgT = sc.sb("sgT", [128, 4, TT], BF16)
    attm = [sc.sb("attm%d" % i, [128, 128], BF16) for i in range(2)]
    Sf = sc.sb("Sf", [128, 2, 128], F32)
    Sb = [sc.sb("Sb%d" % i, [128, 2, 128], BF16) for i in range(2)]
    c.dve.op(lambda e: e.memset(Sf[:], 0.0), writes=[Sf])
    c.dve.op(lambda e: e.memset(Sb[0][:], 0.0), writes=[Sb[0]])
    ss4 = sc.sb("ss4", [128, 4], F32)
    junk = sc.sb("junk", [128, 128], F32)
    on_t = sc.sb("on_t", [128, 512], F32)
    oT = sc.sb("oT", [128, 8, TT], BF16)
    pT = sc.sb("pT", [128, 4, 16 + TT], F32)
    sA = sc.sb("sA", [128, 4, 16 + TT], F32)
    sB = sc.sb("sB", [128, 4, 16 + TT], F32)
    pooled = sc.sb("pooled", [128, 4, TT], BF16)
    c.dve.op(lambda e: e.memset(pT[:], 0.0), writes=[pT])

    import os
    CUT = float(os.environ.get('CUT', '99'))

    def body_mix(t):
        if CUT >= 11: ws.push(ffn_items(2))
        rmsnorm_fm(fb[3], hT, 2, hn)
        for m in range(2):
            for which, dst, scl in ((0, qT, 0.125), (1, kT, 1.0)):
                pb = P[(2 * m + which) % 2]
                col = which * 256 + m * 128
                for cc in range(8):
                    c.pe.op(lambda e, cc=cc, pb=pb, col=col: e.matmul(pb[:, :], lhsT=win0[:, cc, col:col + 128], rhs=hn[:, cc, :], start=(cc == 0), stop=(cc == 7)),
                            reads=[win0, hn], writes=[pb])
                c.act.op(lambda e, dst=dst, m=m, pb=pb, scl=scl: e.activation(out=dst[:, m, :], in_=pb[:, :], func=AF.Copy, scale=scl), reads=[pb], writes=[dst])
        if CUT < 1: return
        for cc in range(8):
            c.pe.op(lambda e, cc=cc: e.matmul(P[2][0:16, :], lhsT=win0[:, cc, 1536:1552], rhs=hn[:, cc, :], start=(cc == 0), stop=(cc == 7)),
                    reads=[win0, hn], writes=[P[2]])
        c.act.op(lambda e: e.copy(out=gra[0:16, :], in_=P[2][0:16, :]), reads=[P[2]], writes=[gra])
        if CUT < 2: return
        for g in range(4):
            pb = P[3]
            for cc in range(8):
                c.pe.op(lambda e, cc=cc, g=g, pb=pb: e.matmul(pb[:, :], lhsT=win0[:, cc, 1552 + g * 128:1552 + (g + 1) * 128], rhs=hn[:, cc, :], start=(cc == 0), stop=(cc == 7)),
                        reads=[win0, hn], writes=[pb])
            c.act.op(lambda e, g=g, pb=pb: e.copy(out=pT[:, g, 16:16 + TT], in_=pb[:, :]), reads=[pb], writes=[pT])
        if CUT < 3: return
        W = 16 + TT
        c.pool.op(lambda e: e.tensor_tensor(out=sA[:, 0:4, 1:W], in0=pT[:, 0:4, 1:W], in1=pT[:, 0:4, 0:W - 1], op=ALU.add), reads=[pT], writes=[sA])
        c.pool.op(lambda e: e.tensor_tensor(out=sB[:, 1:4, 3:W], in0=sA[:, 1:4, 3:W], in1=sA[:, 1:4, 1:W - 2], op=ALU.add), reads=[sA], writes=[sB])
        c.pool.op(lambda e: e.tensor_tensor(out=sA[:, 2:4, 7:W], in0=sB[:, 2:4, 7:W], in1=sB[:, 2:4, 3:W - 4], op=ALU.add), reads=[sB], writes=[sA])
        c.pool.op(lambda e: e.tensor_tensor(out=sB[:, 3:4, 15:W], in0=sA[:, 3:4, 15:W], in1=sA[:, 3:4, 7:W - 8], op=ALU.add), reads=[sA], writes=[sB])
        for g in range(4):
            src = sA if g % 2 == 0 else sB
            wdw = 2 ** (g + 1)
            c.dve.op(lambda e, g=g, src=src, wdw=wdw: e.scalar_tensor_tensor(out=pooled[:, g, :], in0=src[:, g, 16:W], scalar=1.0 / wdw, op0=ALU.mult,
                                                                             in1=pT[:, g, 16:W], op1=ALU.subtract),
                     reads=[src, pT], writes=[pooled])
            if t == 0:
                n = wdw - 1
                c.dve.op(lambda e, g=g, src=src, n=n: e.tensor_tensor(out=junk[:, 0:n], in0=src[:, g, 16:16 + n], in1=invc[:, 0:n], op=ALU.mult),
                         reads=[src, invc], writes=[junk])
                c.dve.op(lambda e, g=g, n=n: e.tensor_tensor(out=pooled[:, g, 0:n], in0=junk[:, 0:n], in1=pT[:, g, 16:16 + n], op=ALU.subtract),
                         reads=[junk, pT], writes=[pooled])
        c.pool.op(lambda e: e.tensor_copy(out=sA[:, :, 0:16], in_=pT[:, :, TT:TT + 16]), reads=[pT], writes=[sA])
        c.pool.op(lambda e: e.tensor_copy(out=pT[:, :, 0:16], in_=sA[:, :, 0:16]), reads=[sA], writes=[pT])
        for g in range(4):
            pb = P[3]
            c.pe.op(lambda e, g=g, pb=pb: e.matmul(pb[:, :], lhsT=poolw[:, g, :], rhs=pooled[:, g, :], start=True, stop=True), reads=[poolw, pooled], writes=[pb])
            c.act.op(lambda e, g=g, pb=pb: e.activation(out=oT[:, 4 + g, :], in_=pb[:, :], func=AF.Identity, scale=pscale[:, g:g + 1]), reads=[pb, pscale], writes=[oT])
        for h in range(4):
            pb = P[4 + h % 2]
            for cc in range(8):
                c.pe.op(lambda e, cc=cc, h=h, pb=pb: e.matmul(pb[:, :], lhsT=win0[:, cc, 1024 + h * 128:1024 + (h + 1) * 128], rhs=hn[:, cc, :], start=(cc == 0), stop=(cc == 7)),
                        reads=[win0, hn], writes=[pb])
            c.act.op(lambda e, h=h, pb=pb: e.activation(out=sgT[:, h, :], in_=pb[:, :], func=AF.Silu), reads=[pb], writes=[sgT])
        if CUT < 4: return
        for s in range(int(os.environ.get('NSUB', '4'))):
            ts = slice(s * 128, (s + 1) * 128)
            c.pe.op(lambda e, ts=ts: e.matmul(P[0][:, 0:256], lhsT=gra[0:33, ts], rhs=w2a[0:33, :], start=True, stop=True), reads=[gra, w2a], writes=[P[0]])
            c.act.op(lambda e: e.activation(out=lsp[:], in_=P[0][:, 0:256], func=AF.Exp, scale=-1.0), reads=[P[0]], writes=[lsp])
            c.act.op(lambda e: e.activation(out=lsp[:], in_=lsp[:], func=AF.Ln, bias=oneb[:], scale=1.0), reads=[lsp, oneb], writes=[lsp])
            if CUT < 5: continue
            for m in range(2):
                c.pe.op(lambda e, m=m: e.matmul(P[1][:, m * 128:(m + 1) * 128], lhsT=lsp[:, m * 128:(m + 1) * 128], rhs=ltri[:], start=True, stop=True),
                        reads=[lsp, ltri], writes=[P[1]])
            c.pe.op(lambda e: e.matmul(P[0][:, 256:512], lhsT=mgt[:], rhs=lsp[:], start=True, stop=True), reads=[lsp, mgt], writes=[P[0]])
            c.act.op(lambda e: e.activation(out=ebT[:].rearrange("p m i -> p (m i)"), in_=P[1][:, 0:256], func=AF.Exp), reads=[P[1]], writes=[ebT])
            c.act.op(lambda e: e.activation(out=enbT[:].rearrange("p m i -> p (m i)"), in_=P[1][:, 0:256], func=AF.Exp, scale=-1.0), reads=[P[1]], writes=[enbT])
            c.act.op(lambda e: e.activation(out=ekend[:], in_=P[0][:, 256:512], func=AF.Exp), reads=[P[0]], writes=[ekend])
            c.dve.op(lambda e, ts=ts: e.tensor_tensor(out=qdec[:], in0=qT[:, :, ts], in1=ebT[:], op=ALU.mult), reads=[qT, ebT], writes=[qdec])
            c.dve.op(lambda e, ts=ts: e.tensor_tensor(out=kinv[:], in0=kT[:, :, ts], in1=enbT[:], op=ALU.mult), reads=[kT, enbT], writes=[kinv])
            if CUT < 6: continue
            for cc in range(8):
                c.pe.op(lambda e, cc=cc, ts=ts: e.matmul(P[2][:, 0:256], lhsT=hn[:, cc, ts], rhs=win0[:, cc, 256:512], start=(cc == 0), stop=(cc == 7)),
                        reads=[hn, win0], writes=[P[2]])
            c.dve.op(lambda e: e.tensor_tensor(out=kend[:], in0=P[2][:, 0:256], in1=ekend[:], op=ALU.mult), reads=[P[2], ekend], writes=[kend])
            if CUT < 6.3: continue
            for cc in range(8):
                c.pe.op(lambda e, cc=cc, ts=ts: e.matmul(P[3][:, :], lhsT=hn[:, cc, ts], rhs=win0[:, cc, 512:1024], start=(cc == 0), stop=(cc == 7)),
                        reads=[hn, win0], writes=[P[3]])
            c.act.op(lambda e: e.copy(out=vtok[:], in_=P[3][:, :]), reads=[P[3]], writes=[vtok])
            DSB = (P[5], P[1])
            for ch in range(2):
                tb = slice(ch * 64, (ch + 1) * 64)
                for h in range(4):
                    m, po = h // 2, (h % 2) * 64
                    c.pe.op(lambda e, ch=ch, tb=tb, h=h, m=m, po=po: e.matmul(DSB[ch][po:po + 64, 256 + m * 128:256 + (m + 1) * 128],
                                                                             lhsT=kend[tb, h * 64:(h + 1) * 64], rhs=vtok[tb, h * 128:(h + 1) * 128], start=True, stop=True),
                            reads=[kend, vtok], writes=[DSB[ch]])
            if CUT < 8: continue
            for ch in range(2):
                for m in range(2):
                    c.dve.op(lambda e, ch=ch, m=m: e.scalar_tensor_tensor(out=Sf[:, m, :], in0=Sf[:, m, :], scalar=ebT[:, m, ch * 64 + 63:ch * 64 + 64], op0=ALU.mult,
                                                                          in1=DSB[ch][:, 256 + m * 128:256 + (m + 1) * 128], op1=ALU.add),
                             reads=[Sf, ebT, DSB[ch]], writes=[Sf])
                if ch == 0:
                    c.act.op(lambda e: e.copy(out=Sb[1][:], in_=Sf[:]), reads=[Sf], writes=[Sb[1]])
            if CUT < 9: continue
            for h in range(4):
                m, po = h // 2, (h % 2) * 64
                pa = P[6 + h % 2] if False else P[6]
                am = attm[h % 2]
                c.pe.op(lambda e, m=m, po=po, pa=pa: e.matmul(pa[:, 0:128], lhsT=kinv[po:po + 64, m, :], rhs=qdec[po:po + 64, m, :], start=True, stop=True),
                        reads=[kinv, qdec], writes=[pa])
                c.dve.op(lambda e, pa=pa, am=am: e.tensor_tensor(out=am[:], in0=pa[:, 0:128], in1=mask01[:], op=ALU.mult), reads=[pa, mask01], writes=[am])
                hc = slice(h * 128, (h + 1) * 128)
                c.pe.op(lambda e, am=am, hc=hc: e.matmul(P[7][:, hc], lhsT=am[:], rhs=vtok[:, hc], start=True, stop=False, skip_group_check=True), reads=[am, vtok], writes=[P[7]])
                c.pe.op(lambda e, m=m, po=po, hc=hc: e.matmul(P[7][0:64, hc], lhsT=qdec[po:po + 64, m, 0:64], rhs=Sb[0][po:po + 64, m, :], start=False, stop=True, skip_group_check=True),
                        reads=[qdec, Sb[0]], writes=[P[7]])
                c.pe.op(lambda e, m=m, po=po, hc=hc: e.matmul(P[7][64:128, hc], lhsT=qdec[po:po + 64, m, 64:128], rhs=Sb[1][po:po + 64, m, :], start=False, stop=True, skip_group_check=True),
                        reads=[qdec, Sb[1]], writes=[P[7]])
            c.act.op(lambda e: e.copy(out=Sb[0][:], in_=Sf[:]), reads=[Sf], writes=[Sb[0]])
            if CUT < 10: continue
            for h in range(4):
                hc = slice(h * 128, (h + 1) * 128)
                c.act.op(lambda e, h=h, hc=hc: e.activation(out=junk[:], in_=P[7][:, hc], func=AF.Square, accum_out=ss4[:, h:h + 1]), reads=[P[7]], writes=[junk, ss4])
            c.act.op(lambda e: e.activation(out=ss4[:], in_=ss4[:], func=AF.Ln, bias=epsb[:], scale=1.0 / 128), reads=[ss4, epsb], writes=[ss4])
            c.act.op(lambda e: e.activation(out=ss4[:], in_=ss4[:], func=AF.Exp, scale=-0.5), reads=[ss4], writes=[ss4])
            for h in range(4):
                hc = slice(h * 128, (h + 1) * 128)
                c.dve.op(lambda e, h=h, hc=hc: e.scalar_tensor_tensor(out=on_t[:, hc], in0=P[7][:, hc], scalar=ss4[:, h:h + 1], op0=ALU.mult, in1=gnb[:], op1=ALU.mult),
                         reads=[P[7], ss4, gnb], writes=[on_t])
            if CUT < 10: continue
            for h in range(4):
                c.pe.op(lambda e, h=h: e.transpose(out=P[4][:, h * 128:(h + 1) * 128], in_=on_t[:, h * 128:(h + 1) * 128], identity=identf[:]),
                        reads=[on_t, identf], writes=[P[4]])
            c.dve.op(lambda e, ts=ts: e.tensor_tensor(out=oT[:, 0:4, ts], in0=P[4][:, :].rearrange("p (h i) -> p h i", h=4), in1=sgT[:, :, ts], op=ALU.mult),
                     reads=[P[4], sgT], writes=[oT])
        if CUT < 10: return
        for cc in range(8):
            pb = P[cc % 2]
            for k in range(8):
                c.pe.op(lambda e, cc=cc, k=k, pb=pb: e.matmul(pb[:, :], lhsT=wout0[:, k, cc * 128:(cc + 1) * 128], rhs=oT[:, k, :], start=(k == 0), stop=(k == 7)),
                        reads=[wout0, oT], writes=[pb])
            c.dve.op(lambda e, cc=cc, pb=pb: e.tensor_tensor(out=hT[:, cc, :], in0=pb[:, :], in1=hT[:, cc, :], op=ALU.add), reads=[pb, hT], writes=[hT])

        if CUT >= 11: ffn(ws, fb, hT, 4)

    for l0 in range(1):
        for t in range(NT):
            if stage != "X":
                ws.push(ffn_items(0))
            load_x_tile(t, xt, hT, dx)
            if stage == "X":
                store_out_tile(t, hT, xt, dx)
                continue
            ffn(ws, fb, hT, 0)
            if stage == "F":
                store_out_tile(t, hT, xt, dx)
                continue
            if stage == "A":
                body_mix(t)
                store_out_tile(t, hT, xt, dx)
                continue
            body_mix(t)
            c.dma(c.sp, dhs, HS[:, :, t * TT:(t + 1) * TT], hT[:], reads=[hT], writes=[HS])
    sc.close()
    if stage in ("A", "X", "F"):
        c.emit()
        return nc
    NKT = T // 128
    NCOL = T // 16
    NCK = max(1, T // 2048)
    bplan = bias_plan(T)
    biasT = g_sc.sb("biasT", [128, len(bplan)], F32)
    c.dma(c.sp, c.dsem("dbias"), biasT[:], A["biasT"], writes=[biasT])

    def bcol(key):
        i = bplan[key]
        return biasT[:, i:i + 1]

    sc = Scope(c)
    ws = WStream(c, sc, 3, 2816)
    fb = ffn_bufs(sc)
    hn = fb[0]
    hT = sc.sb("hT", [128, 8, TT], F32)
    xt = sc.sb("xt", [128, 4, D], F32)
    dx = c.dsem("dxB")
    dhl = c.dsem("dhlB")
    dhs2 = c.dsem("dhsB")
    win1 = load_resident(sc, "win1", A["c_w_in"], IN1)
    qst = sc.sb("qst", [64, 16, TT], BF16)
    kst = {nm: sc.sb("kst_" + nm, [64, 2, TT], BF16) for nm in ("kc", "vc", "ks", "kw")}
    vfm = sc.sb("vfm", [128, TT], BF16)
    vst = {nm: sc.sb("vst_" + nm, [128, 4, 128], BF16) for nm in ("vs", "vw")}
    gfm = sc.sb("gfm", [48, TT], F32)
    gst = sc.sb("gst", [128, 4, 48], F32)
    dst_ = {nm: c.dsem("dst_" + nm) for nm in ("q", "kc", "vc", "ks", "kw", "vs", "vw", "g")}
    KCOL = {"kc": 1024, "vc": 1152, "ks": 1280, "vs": 1408, "kw": 1536, "vw": 1664}
    for t in range(NT):
        tsl = slice(t * TT, (t + 1) * TT)
        ws.push(ffn_items(1))
        c.dma(c.sp, dhl, hT[:], HS[:, :, tsl], reads=[HS], writes=[hT])
        ffn(ws, fb, hT, 1)
        if stage == "B":
            store_out_tile(t, hT, xt, dx)
            continue
        c.dma(c.sp, dhs2, HS[:, :, tsl], hT[:], reads=[hT], writes=[HS])
        rmsnorm_fm(fb[3], hT, 3, hn)
        for m in range(8):
            pb = P[m % 2]
            for cc in range(8):
                c.pe.op(lambda e, cc=cc, m=m, pb=pb: e.matmul(pb[:, :], lhsT=win1[:, cc, m * 128:(m + 1) * 128], rhs=hn[:, cc, :], start=(cc == 0), stop=(cc == 7)),
                        reads=[win1, hn], writes=[pb])
            c.dve.op(lambda e, m=m, pb=pb: e.tensor_scalar(out=qst[:, 2 * m, :], in0=pb[0:64, :], scalar1=0.125, scalar2=None, op0=ALU.mult), reads=[pb], writes=[qst])
            c.dve.op(lambda e, m=m, pb=pb: e.tensor_scalar(out=qst[:, 2 * m + 1, :], in0=pb[64:128, :], scalar1=0.125, scalar2=None, op0=ALU.mult), reads=[pb], writes=[qst])
        c.dma(c.sp, dst_["q"], QS[:, :, tsl], qst[:], reads=[qst], writes=[QS])
        for i, nm in enumerate(("kc", "vc", "ks", "kw")):
            pb = P[2 + i % 2]
            col = KCOL[nm]
            for cc in range(8):
                c.pe.op(lambda e, cc=cc, col=col, pb=pb: e.matmul(pb[:, :], lhsT=win1[:, cc, col:col + 128], rhs=hn[:, cc, :], start=(cc == 0), stop=(cc == 7)),
                        reads=[win1, hn], writes=[pb])
            c.dve.op(lambda e, nm=nm, pb=pb: e.tensor_copy(out=kst[nm][:, 0, :], in_=pb[0:64, :]), reads=[pb], writes=[kst[nm]])
            c.dve.op(lambda e, nm=nm, pb=pb: e.tensor_copy(out=kst[nm][:, 1, :], in_=pb[64:128, :]), reads=[pb], writes=[kst[nm]])
            c.dma(c.sp, dst_[nm], KSs[nm][:, :, tsl], kst[nm][:], reads=[kst[nm]], writes=[KSs[nm]])
        for i, nm in enumerate(("vs", "vw")):
            pb = P[4 + i]
            col = KCOL[nm]
            for cc in range(8):
                c.pe.op(lambda e, cc=cc, col=col, pb=pb: e.matmul(pb[:, :], lhsT=win1[:, cc, col:col + 128], rhs=hn[:, cc, :], start=(cc == 0), stop=(cc == 7)),
                        reads=[win1, hn], writes=[pb])
            c.act.op(lambda e, pb=pb: e.copy(out=vfm[:], in_=pb[:, :]), reads=[pb], writes=[vfm])
            ptb = P[7].ap.bitcast(BF16)
            for s4 in range(4):
                c.pe.op(lambda e, s4=s4, ptb=ptb: e.transpose(out=ptb[:, s4 * 128:(s4 + 1) * 128], in_=vfm[:, s4 * 128:(s4 + 1) * 128], identity=identb[:]),
                        reads=[vfm, identb], writes=[P[7]])
            c.dve.op(lambda e, nm=nm, ptb=ptb: e.tensor_copy(out=vst[nm][:].rearrange("p s f -> p (s f)"), in_=ptb[:, 0:512]), reads=[P[7]], writes=[vst[nm]])
            c.dma(c.sp, dst_[nm], VSs[nm][tsl, :, :].rearrange("(s p) g d -> p s (g d)", p=128), vst[nm][:], reads=[vst[nm]], writes=[VSs[nm]])
        for cc in range(8):
            c.pe.op(lambda e, cc=cc: e.matmul(P[6][0:48, :], lhsT=win1[:, cc, 1792:1840], rhs=hn[:, cc, :], start=(cc == 0), stop=(cc == 7)),
                    reads=[win1, hn], writes=[P[6]])
        c.act.op(lambda e: e.activation(out=gfm[:], in_=P[6][0:48, :], func=AF.Sigmoid), reads=[P[6]], writes=[gfm])
        for s4 in range(4):
            c.pe.op(lambda e, s4=s4: e.transpose(out=P[7][:, 256 + s4 * 48:256 + (s4 + 1) * 48], in_=gfm[0:48, s4 * 128:(s4 + 1) * 128], identity=identf[0:48, 0:48]),
                    reads=[gfm, identf], writes=[P[7]])
        c.act.op(lambda e: e.copy(out=gst[:].rearrange("p s f -> p (s f)"), in_=P[7][:, 256:256 + 192]), reads=[P[7]], writes=[gst])
        c.dma(c.sp, dst_["g"], GS[tsl, :].rearrange("(s p) f -> p s f", p=128), gst[:], reads=[gst], writes=[GS])
    sc.close()
    if stage == "B":
        c.emit()
        return nc

    pd = Scope(c)
    KC = pd.sb("KC", [67, 2, NCK * 128], BF16)
    VCM = pd.sb("VCM", [128, NCK, 2, 65], BF16)
    c.dve.op(lambda e: e.memset(KC[:], 0.0), writes=[KC])
    c.dve.op(lambda e: e.memset(VCM[:], 0.0), writes=[VCM])
    c.dve.op(lambda e: e.memset(VCM[:, :, :, 64:65], 1.0), writes=[VCM])
    c.dma(c.sp, c.dsem("dkcaug"), KC[64:67, :, :], A["kcaugT"], writes=[KC])
    sc = Scope(c)
    stk = sc.sb("stk", [128, T + 16], BF16)
    w1 = sc.sb("w1", [128, 16, 256], BF16)
    w2 = sc.sb("w2", [128, 2, 64], BF16)
    pef = sc.sb("pef", [128, 16], BF16)
    pebias = sc.sb("pebias", [128, 2], F32)
    xg = sc.sb("xg", [128, 512], F32)
    x2 = sc.sb("x2", [128, 512], F32)
    sgm = sc.sb("sgm", [128, 512], F32)
    hg = sc.sb("hg", [128, 2, 512], BF16)
    c.dma(c.pool, c.dsem("dpef"), pef[:], A["pef"], writes=[pef])
    dstk = c.dsem("dstk")
    dw1 = c.dsem("dw1")
    for zi, (znm, w1n, w2n) in enumerate((("kc", "cmpk_w1", "cmpk_w2"), ("vc", "cmpv_w1", "cmpv_w2"))):
        c.dma(c.pool, dw1, [w1[:], w2[:]], [A[w1n].rearrange("(m p) c -> p m c", p=128), A[w2n].rearrange("(k p) d -> p k d", p=128)], writes=[w1, w2])
        for cc in range(2):
            for m in range(16):
                c.pe.op(lambda e, cc=cc, m=m: e.matmul(P[2][:, cc:cc + 1], lhsT=w1[:, m, cc * 128:(cc + 1) * 128], rhs=pef[:, m:m + 1], start=(m == 0), stop=(m == 15)),
                        reads=[w1, pef], writes=[P[2]])
        c.act.op(lambda e: e.copy(out=pebias[:], in_=P[2][:, 0:2]), reads=[P[2]], writes=[pebias])
        for g in range(2):
            c.dve.op(lambda e: e.memset(stk[:], 0.0), writes=[stk])
            c.dma(c.sp, dstk, [stk[0:64, 0:T], stk[64:128, 0:T - 1]], [KSs[znm][:, g, 0:T], KSs[znm][:, g, 1:T]], reads=[KSs[znm]], writes=[stk])
            for cc in range(2):
                for m in range(16):
                    c.pe.op(lambda e, cc=cc, m=m: e.matmul(P[cc][:, 0:NCOL], lhsT=w1[:, m, cc * 128:(cc + 1) * 128], rhs=stk[:, 2 * m:2 * m + 16 * (NCOL - 1) + 1:16], start=(m == 0), stop=(m == 15)),
                            reads=[w1, stk], writes=[P[cc]])
                c.act.op(lambda e, cc=cc: e.activation(out=xg[:, 0:NCOL], in_=P[cc][:, 0:NCOL], func=AF.Identity, bias=pebias[:, cc:cc + 1], scale=1.0), reads=[P[cc], pebias], writes=[xg])
                c.dve.op(lambda e: e.tensor_tensor(out=x2[:, 0:NCOL], in0=xg[:, 0:NCOL], in1=xg[:, 0:NCOL], op=ALU.mult), reads=[xg], writes=[x2])
                c.dve.op(lambda e: e.tensor_scalar(out=x2[:, 0:NCOL], in0=x2[:, 0:NCOL], scalar1=0.044715, scalar2=1.0, op0=ALU.mult, op1=ALU.add), reads=[x2], writes=[x2])
                c.dve.op(lambda e: e.tensor_tensor(out=x2[:, 0:NCOL], in0=x2[:, 0:NCOL], in1=xg[:, 0:NCOL], op=ALU.mult), reads=[x2, xg], writes=[x2])
                c.act.op(lambda e: e.activation(out=sgm[:, 0:NCOL], in_=x2[:, 0:NCOL], func=AF.Sigmoid, scale=1.5957691216057308), reads=[x2], writes=[sgm])
                c.dve.op(lambda e, cc=cc: e.tensor_tensor(out=hg[:, cc, 0:NCOL], in0=xg[:, 0:NCOL], in1=sgm[:, 0:NCOL], op=ALU.mult), reads=[xg, sgm], writes=[hg])
            if zi == 0:
                for cc in range(2):
                    c.pe.op(lambda e, cc=cc: e.matmul(P[3][0:64, 0:NCOL], lhsT=w2[:, cc, :], rhs=hg[:, cc, 0:NCOL], start=(cc == 0), stop=(cc == 1)), reads=[w2, hg], writes=[P[3]])
                c.act.op(lambda e, g=g: e.copy(out=KC[0:64, g, 0:NCOL], in_=P[3][0:64, 0:NCOL]), reads=[P[3]], writes=[KC])
            else:
                for nb in range((NCOL + 127) // 128):
                    cols = min(128, NCOL - nb * 128)
                    for cc in range(2):
                        c.pe.op(lambda e, cc=cc, nb=nb, cols=cols: e.matmul(P[4][0:cols, 0:64], lhsT=hg[:, cc, nb * 128:nb * 128 + cols], rhs=w2[:, cc, :], start=(cc == 0), stop=(cc == 1)),
                                reads=[w2, hg], writes=[P[4]])
                    c.act.op(lambda e, g=g, nb=nb, cols=cols: e.copy(out=VCM[0:cols, nb, g, 0:64], in_=P[4][0:cols, 0:64]), reads=[P[4]], writes=[VCM])
    sc.close()

    sc = Scope(c)
    KS = sc.sb("KS", [67, 2, T], BF16)
    VS = sc.sb("VS", [128, NKT, 2, 65], BF16)
    c.dve.op(lambda e: e.memset(VS[:, :, :, 64:65], 1.0), writes=[VS])
    c.dma(c.sp, c.dsem("dKS"), [KS[0:64, :, :], KS[64:67, :, :]], [KSs["ks"][:, :, :], A["kaugT"]], reads=[KSs["ks"]], writes=[KS])
    dVS = c.dsem("dVS")
    step = 16
    c.dma(c.sp, dVS, [VS[:, k0:min(NKT, k0 + step), g, 0:64] for k0 in range(0, NKT, step) for g in range(2)],
          [VSs["vs"][k0 * 128:min(NKT, k0 + step) * 128, g, :].rearrange("(k p) d -> p k d", p=128) for k0 in range(0, NKT, step) for g in range(2)],
          reads=[VSs["vs"]], writes=[VS])
    QA = [sc.sb("QA%d" % i, [67, 16, TT], BF16) for i in range(2)]
    KWt = [sc.sb("KWt%d" % i, [67, 2, 1024], BF16) for i in range(2)]
    VWt = [sc.sb("VWt%d" % i, [128, 8, 2, 65], BF16) for i in range(2)]
    for i in range(2):
        c.dve.op(lambda e, i=i: e.memset(VWt[i][:, :, :, 64:65], 1.0), writes=[VWt[i]])
        c.dma(c.sp, c.dsem("dqaug%d" % i), [QA[i][64:67, :, :], KWt[i][64:67, :, :]], [A["qaug"], A["kaugT"][:, :, 0:1024]], writes=[QA[i], KWt[i]])
    dQA = [c.dsem("dQA%d" % i) for i in range(2)]
    dKW = [c.dsem("dKW%d" % i) for i in range(2)]
    dVW = [c.dsem("dVW%d" % i) for i in range(2)]
    gt = sc.sb("gt", [128, 4, 48], F32)
    dgt = c.dsem("dgt")
    oacc = sc.sb("oacc", [128, 4, D], F32)
    impacc = sc.sb("impacc", [128, 4, 128], F32)
    impm = sc.sb("impm", [128, 128], F32)
    rep = sc.sb("rep", [128, 128], F32)
    mx8 = sc.sb("mx8", [128, 16], F32)
    thr = sc.sb("thr", [128, 1], F32)
    selB = sc.sb("selB", [128, 128], BF16)
    selT = sc.sb("selT", [128, TT], BF16)
    PT = [sc.sb("PT%d" % i, [128, TT], BF16) for i in range(5)]
    rs = sc.sb("rs", [128, 4], F32)
    fac = sc.sb("fac", [128, 4], F32)
    cmask = sc.sb("cmask", [128, 4, TT], BF16)
    wmask = sc.sb("wmask", [128, 4, TT], BF16)
    pmask = sc.sb("pmask", [128, 5, TT], BF16)
    ew = sc.sb("ew", [128, 32, 128], BF16)
    mcs = sc.sb("mcs", [128, 4, 128], BF16)
    cb = sc.sb("cb", [128, 256], F32)
    oTn = sc.sb("oTn", [128, 8, TT], BF16)
    dOS = c.dsem("dOS")
    c.dma(c.sp, c.dsem("dmasks"), [cmask[:], wmask[:], pmask[:], ew[:], mcs[:], cb[:]], [A["cmask"], A["wmask"], A["pmask"], A["ew"], A["mcs"], A["cb"]],
          writes=[cmask, wmask, pmask, ew, mcs, cb], n=6)
    pti = [0]
    psi = [0]
    poi = [0]
    pipe = Pipe(2)
    sbanks = [[P[0], P[1], P[2]]]

    def score_tile(lhs_k, qa_h, extra, bias_ap, cr=(0, TT)):
        c0, c1 = cr
        ps = sbanks[0][psi[0] % len(sbanks[0])]
        psi[0] += 1
        n_mm = 1 + len(extra)
        kt_tk, k_ap = lhs_k
        qa_tk, q_ap = qa_h
        c.pe.op(lambda e: e.matmul(ps[:, c0:c1], lhsT=k_ap, rhs=q_ap[:, c0:c1], start=True, stop=(n_mm == 1)), reads=[kt_tk, qa_tk], writes=[ps])
        for i, (l_ap, r_ap, rds) in enumerate(extra):
            c.pe.op(lambda e, l_ap=l_ap, r_ap=r_ap, last=(i == len(extra) - 1): e.matmul(ps[:, c0:c1], lhsT=l_ap, rhs=r_ap[:, c0:c1], start=False, stop=last), reads=rds, writes=[ps])
        pt = PT[pti[0] % 5]
        pti[0] += 1
        c.act.op(lambda e: e.activation(out=pt[:, c0:c1], in_=ps[:, c0:c1], func=AF.Exp, bias=bias_ap, scale=1.0), reads=[ps, biasT], writes=[pt])
        return pt

    def finish_head(po, h, br, first):
        c.dve.op(lambda e, po=po: e.tensor_scalar(out=rs[:], in0=po[:, 0:260].rearrange("p (s f) -> p s f", f=65)[:, :, 64], scalar1=1e-30, scalar2=None, op0=ALU.add), reads=[po], writes=[rs])
        c.dve.op(lambda e: e.reciprocal(out=rs[:], in_=rs[:]), reads=[rs], writes=[rs])
        c.dve.op(lambda e, h=h, br=br: e.tensor_tensor(out=fac[:], in0=rs[:], in1=gt[:, :, 3 * h + br], op=ALU.mult), reads=[rs, gt], writes=[fac])
        for s4 in range(4):
            if first:
                c.dve.op(lambda e, po=po, s4=s4, h=h: e.tensor_scalar(out=oacc[:, s4, h * 64:(h + 1) * 64], in0=po[:, s4 * 65:s4 * 65 + 64], scalar1=fac[:, s4:s4 + 1], scalar2=None, op0=ALU.mult),
                         reads=[po, fac], writes=[oacc])
            else:
                c.dve.op(lambda e, po=po, s4=s4, h=h: e.scalar_tensor_tensor(out=oacc[:, s4, h * 64:(h + 1) * 64], in0=po[:, s4 * 65:s4 * 65 + 64], scalar=fac[:, s4:s4 + 1], op0=ALU.mult,
                                                                             in1=oacc[:, s4, h * 64:(h + 1) * 64], op1=ALU.add),
                         reads=[po, fac, oacc], writes=[oacc])

    def pv(po, pt, v_tk, v_ap, subs, started):
        for s4 in subs:
            st = not started[0]
            started[0] = True
            c.pe.op(lambda e, po=po, pt=pt, v_ap=v_ap, s4=s4, st=st: e.matmul(po[:, s4 * 65:(s4 + 1) * 65], lhsT=pt[:, s4 * 128:(s4 + 1) * 128], rhs=v_ap, start=st, stop=True, skip_group_check=True),
                    reads=[pt, v_tk], writes=[po])

    for qt in range(NT):
        t0 = qt * TT
        b = qt % 2
        tsl = slice(t0, t0 + TT)
        c.dma(c.sp, dQA[b], QA[b][0:64, :, :], QS[:, :, tsl], reads=[QS], writes=[QA[b]])
        c.dma(c.sp, dgt, gt[:], GS[tsl, :].rearrange("(s p) f -> p s f", p=128), reads=[GS], writes=[gt])
        lo = max(0, t0 - 512)
        jlo = (lo - (t0 - 512)) // 128
        c.dma(c.sp, dKW[b], KWt[b][0:64, :, jlo * 128:1024], KSs["kw"][:, :, lo:t0 + 512], reads=[KSs["kw"]], writes=[KWt[b]])
        c.dma(c.sp, dVW[b], [VWt[b][:, jlo:8, g, 0:64] for g in range(2)], [VSs["vw"][lo:t0 + 512, g, :].rearrange("(k p) d -> p k d", p=128) for g in range(2)], reads=[VSs["vw"]], writes=[VWt[b]])
        for g in range(2):
            pipe.depth = 2
            sbanks[0] = [P[0], P[1], P[2]]
            nmax = (t0 + 511 - 31) // 16
            nkc = nmax // 128 + 1
            for hh in range(8):
                h = g * 8 + hh
                po = P[3 + poi[0] % 2]
                pim = P[5 + poi[0] % 2]
                poi[0] += 1
                st_o = [False]
                st_i = [False]
                for ktc in range(nkc):
                    off = t0 - 2048 * ktc
                    extra = []
                    if 2048 + 15 > off:
                        v = off // 512
                        extra.append((identb[:], pmask[:, v, :], [identb, pmask]))
                    pt = score_tile((KC, KC[0:67, g, ktc * 128:(ktc + 1) * 128]), (QA[b], QA[b][0:67, h, :]), extra, bcol(("c", h, off - 31)))

                    def _cpv(po=po, pim=pim, pt=pt, ktc=ktc, g=g, st_o=st_o, st_i=st_i):
                        pv(po, pt, VCM, VCM[:, ktc, g, :], range(4), st_o)
                        for s4 in range(4):
                            st = not st_i[0]
                            st_i[0] = True
                            c.pe.op(lambda e, s4=s4, st=st: e.matmul(pim[:, s4 * 128:(s4 + 1) * 128], lhsT=pt[:, s4 * 128:(s4 + 1) * 128], rhs=mcs[:, ktc, :], start=st, stop=True, skip_group_check=True),
                                    reads=[pt, mcs], writes=[pim])
                    pipe.push(_cpv)

                def _cfin(po=po, pim=pim, h=h, hh=hh):
                    finish_head(po, h, 0, True)
                    for s4 in range(4):
                        if hh == 0:
                            c.dve.op(lambda e, s4=s4: e.tensor_scalar(out=impacc[:, s4, :], in0=pim[:, s4 * 128:(s4 + 1) * 128], scalar1=rs[:, s4:s4 + 1], scalar2=None, op0=ALU.mult),
                                     reads=[pim, rs], writes=[impacc])
                        else:
                            c.dve.op(lambda e, s4=s4: e.scalar_tensor_tensor(out=impacc[:, s4, :], in0=pim[:, s4 * 128:(s4 + 1) * 128], scalar=rs[:, s4:s4 + 1], op0=ALU.mult, in1=impacc[:, s4, :], op1=ALU.add),
                                     reads=[pim, rs, impacc], writes=[impacc])
                pipe.push(_cfin)
            pipe.flush()
            for s4 in range(4):
                c0 = (t0 + 128 * s4) // 64
                c.dve.op(lambda e, s4=s4, c0=c0: e.tensor_tensor(out=impm[:], in0=impacc[:, s4, :], in1=cb[:, 128 - c0:256 - c0], op=ALU.add), reads=[impacc, cb], writes=[impm])
                c.dve.op(lambda e: e.tensor_scalar(out=impm[:, 0:1], in0=impm[:, 0:1], scalar1=1e4, scalar2=None, op0=ALU.add), reads=[impm], writes=[impm])
                c.dve.op(lambda e: e.max(out=mx8[:, 0:8], in_=impm[:]), reads=[impm], writes=[mx8])
                c.dve.op(lambda e: e.match_replace(out=rep[:], in_to_replace=mx8[:, 0:8], in_values=impm[:], imm_value=-1e30), reads=[impm, mx8], writes=[rep])
                c.dve.op(lambda e: e.max(out=mx8[:, 8:16], in_=rep[:]), reads=[rep], writes=[mx8])
                c.dve.op(lambda e: e.tensor_scalar(out=thr[:], in0=mx8[:, 15:16], scalar1=-0.5, scalar2=None, op0=ALU.max), reads=[mx8], writes=[thr])
                c.dve.op(lambda e: e.tensor_scalar(out=selB[:], in0=impm[:], scalar1=thr[:, 0:1], scalar2=-BIG, op0=ALU.is_lt, op1=ALU.mult), reads=[impm, thr], writes=[selB])
                ptb = P[7].ap.bitcast(BF16)
                c.pe.op(lambda e, ptb=ptb: e.transpose(out=ptb[:, 0:128], in_=selB[:], identity=identb[:]), reads=[selB, identb], writes=[P[7]])
                c.act.op(lambda e, s4=s4, ptb=ptb: e.copy(out=selT[:, s4 * 128:(s4 + 1) * 128], in_=ptb[:, 0:128]), reads=[P[7]], writes=[selT])
            pipe.depth = 4
            sbanks[0] = [P[0], P[1], P[2], P[5], P[6]]
            for hh in range(8):
                h = g * 8 + hh
                po = P[3 + poi[0] % 2]
                poi[0] += 1
                st_o = [False]
                for kt in range(4 * qt + 4):
                    w = (2 * kt) // 64
                    pt_i = ((2 * kt) % 64) // 2
                    extra = [(ew[64 * w:64 * w + 64, pt_i, :], selT[64 * w:64 * w + 64, :], [ew, selT])]
                    jd = kt - 4 * qt
                    if jd >= 0:
                        extra.append((identb[:], cmask[:, jd, :], [identb, cmask]))
                    pt = score_tile((KS, KS[0:67, g, kt * 128:(kt + 1) * 128]), (QA[b], QA[b][0:67, h, :]), extra, bcol(("s", h, 4 * qt - kt)), cr=((jd * 128, TT) if jd > 0 else (0, TT)))
                    pipe.push(lambda po=po, pt=pt, kt=kt, g=g, jd=jd, st_o=st_o: pv(po, pt, VS, VS[:, kt, g, :], [s4 for s4 in range(4) if jd <= s4], st_o))
                pipe.push(lambda po=po, h=h: finish_head(po, h, 1, False))
            for hh in range(8):
                h = g * 8 + hh
                po = P[3 + poi[0] % 2]
                poi[0] += 1
                st_o = [False]
                for j in range(8):
                    kt = 4 * qt - 4 + j
                    if kt < 0:
                        continue
                    mk = wmask[:, j, :] if j < 4 else cmask[:, j - 4, :]
                    mtk = wmask if j < 4 else cmask
                    extra = [(identb[:], mk, [identb, mtk])]
                    pt = score_tile((KWt[b], KWt[b][0:67, g, j * 128:(j + 1) * 128]), (QA[b], QA[b][0:67, h, :]), extra, bcol(("s", h, 4 * qt - kt)), cr=((0, (j + 1) * 128) if j < 4 else ((j - 4) * 128, TT)))
                    subs = [s4 for s4 in range(4) if (s4 <= j if j < 4 else s4 >= j - 4)]
                    pipe.push(lambda po=po, pt=pt, j=j, g=g, b=b, subs=subs, st_o=st_o: pv(po, pt, VWt[b], VWt[b][:, j, g, :], subs, st_o))
                pipe.push(lambda po=po, h=h: finish_head(po, h, 2, False))
            pipe.flush()
        for s4 in range(4):
            for half in range(2):
                pb = P[(2 * s4 + half) % 3]
                for k in range(4):
                    cc = half * 4 + k
                    c.pe.op(lambda e, cc=cc, s4=s4, k=k, pb=pb: e.transpose(out=pb[:, k * 128:(k + 1) * 128], in_=oacc[:, s4, cc * 128:(cc + 1) * 128], identity=identf[:]),
                            reads=[oacc, identf], writes=[pb])
                c.act.op(lambda e, s4=s4, half=half, pb=pb: e.copy(out=oTn[:, half * 4:(half + 1) * 4, s4 * 128:(s4 + 1) * 128], in_=pb[:, :].rearrange("p (k i) -> p k i", k=4)),
                         reads=[pb], writes=[oTn])
        c.dma(c.sp, dOS, OS[:, :, tsl], oTn[:], reads=[oTn], writes=[OS])
    sc.close()
    pd.close()

    sc = Scope(c)
    ws = WStream(c, sc, 3, 2816)
    fb = ffn_bufs(sc)
    hT = sc.sb("hT", [128, 8, TT], F32)
    yT = sc.sb("yT", [128, 8, TT], F32)
    xt = sc.sb("xt", [128, 4, D], F32)
    oTl = sc.sb("oTl", [128, 8, TT], BF16)
    wout1 = load_resident(sc, "wout1", A["c_w_out"], D)
    dx = c.dsem("dxE")
    dhl = c.dsem("dhlE")
    dol = c.dsem("dolE")
    for t in range(NT):
        tsl = slice(t * TT, (t + 1) * TT)
        if stage != "D":
            ws.push(ffn_items(3))
        c.dma(c.sp, dhl, hT[:], HS[:, :, tsl], reads=[HS], writes=[hT])
        c.dma(c.sp, dol, oTl[:], OS[:, :, tsl], reads=[OS], writes=[oTl])
        for cc in range(8):
            pb = P[cc % 2]
            for k in range(8):
                c.pe.op(lambda e, cc=cc, k=k, pb=pb: e.matmul(pb[:, :], lhsT=wout1[:, k, cc * 128:(cc + 1) * 128], rhs=oTl[:, k, :], start=(k == 0), stop=(k == 7)),
                        reads=[wout1, oTl], writes=[pb])
            c.dve.op(lambda e, cc=cc, pb=pb: e.tensor_tensor(out=hT[:, cc, :], in0=pb[:, :], in1=hT[:, cc, :], op=ALU.add), reads=[pb, hT], writes=[hT])
        if stage == "D":
            store_out_tile(t, hT, xt, dx)
            continue
        ffn(ws, fb, hT, 5)
        rmsnorm_fm(fb[3], hT, 6, yT)
        store_out_tile(t, yT, xt, dx)
    sc.close()
    c.emit()
    return nc


def prep_inputs(inp, b, T):
    f = lambda a: np.ascontiguousarray(a, dtype=np.float32)
    m = {}
    m["x"] = f(inp["x"][b, :T])
    m["ffn_wg"] = f(np.stack([inp["ffn1_wg"][0], inp["ffn1_wg"][1], inp["ffn2_wg"][0], inp["ffn2_wg"][1]]))
    m["ffn_wu"] = f(np.stack([inp["ffn1_wu"][0], inp["ffn1_wu"][1], inp["ffn2_wu"][0], inp["ffn2_wu"][1]]))
    m["ffn_wd"] = f(np.stack([inp["ffn1_wd"][0], inp["ffn1_wd"][1], inp["ffn2_wd"][0], inp["ffn2_wd"][1]]))
    gl = [inp["norm_ffn1"][0], inp["norm_ffn1"][1], inp["norm_mix"][0], inp["norm_mix"][1], inp["norm_ffn2"][0], inp["norm_ffn2"][1], inp["final_norm"]]
    m["gam"] = f(np.concatenate([np.asarray(g).reshape(8, 128).T for g in gl], axis=1))
    m["a_w_in"] = f(inp["a_w_in"][0])
    w2a = np.zeros((33, 256), np.float32)
    w2a[0:16] = inp["a_gate_w2"][0]
    w2a[32] = inp["a_gate_b"][0]
    m["w2a"] = w2a
    m["gnorm"] = f(inp["a_gla_norm"][0].reshape(1, 128))
    m["poolw"] = f(inp["a_pool_w"][0])
    m["pscale"] = f(inp["a_pool_scale"][0].reshape(4, 128).T)
    m["a_w_out"] = f(inp["a_w_out"][0])
    m["c_w_in"] = f(inp["c_w_in"][0])
    m["pef"] = f(inp["c_cmp_pe"][0].reshape(16, 128).T)
    for k in ("cmpk_w1", "cmpk_w2", "cmpv_w1", "cmpv_w2"):
        m[k] = f(inp["c_" + k][0])
    m["c_w_out"] = f(inp["c_w_out"][0])
    return m


_CACHE = {}


def run(inputs, T, stage="full", ncores=4):
    inputs = {k: np.asarray(v) for k, v in inputs.items()}
    key = (T, stage)
    if key not in _CACHE:
        _CACHE[key] = build_nc(T, stage)
    nc = _CACHE[key]
    cst = host_consts(T)

    in_maps = []
    shared = None
    for b in range(ncores):
        m = prep_inputs(inputs, b, T)
        if shared is None:
            shared = {k: v for k, v in m.items() if k != "x"}
        else:
            for k in shared:
                m[k] = shared[k]
        for k, v in cst.items():
            m["c_" + k] = v
        m["biasT"] = bias_table(T)
        in_maps.append(m)
    res = run_bass_kernel_spmd(nc, in_maps, core_ids=list(range(ncores)))
    return np.stack([np.asarray(r["out"], dtype=np.float32) for r in res.results], axis=0)


def kernel(**inputs):
    return run(inputs, 8192, "full", 4)
```

```python
import contextlib
import math
import numpy as np
import ml_dtypes
import concourse.bass as bass
import concourse.mybir as mybir
from concourse.bass_utils import run_bass_kernel_spmd

F32 = mybir.dt.float32
BF16 = mybir.dt.bfloat16
AF = mybir.ActivationFunctionType
ALU = mybir.AluOpType
SEM_LIMIT = 30000

D = 1024
DFF = 2816
NF = DFF // 128
TT = 512
EPS = 1e-6
IN0 = 2064
IN1 = 1840
BIG = 30000.0
NHEAD = 16
SLOPES = [2.0 ** (-8.0 * (i + 1) / 16) for i in range(16)]
def head_slope(h):
    g, hh = divmod(h, 8)
    return SLOPES[hh * 2 + g]


class Tk:
    __slots__ = ("ap", "lw", "rd", "name")

    def __init__(self, ap, name=""):
        self.ap = ap
        self.lw = None
        self.rd = {}
        self.name = name

    def __getitem__(self, idx):
        return self.ap[idx]


class _Rec:
    def __init__(self):
        self.call = None

    def __getattr__(self, name):
        def f(*a, **k):
            self.call = (name, a, k)
            return self
        return f


class Eng:
    def __init__(self, ctx, name, is_pe=False):
        self.ctx = ctx
        self.name = name
        self.is_pe = is_pe
        self.sem = ctx.nc.alloc_semaphore(name + "_s0")
        self.epoch = 0
        self.count = 0
        self.waited = {}
        self.prog = []

    def _wait(self, tok):
        if tok is None:
            return
        sem, val, eng = tok
        if eng is self and self.is_pe:
            return
        k = id(sem)
        if self.waited.get(k, 0) >= val:
            return
        self.prog.append(lambda e, sem=sem, val=val: e.wait_ge(sem, val))
        self.waited[k] = val

    def deps(self, reads, writes):
        for t in reads:
            self._wait(t.lw)
        for t in writes:
            self._wait(t.lw)
            for tok in t.rd.values():
                self._wait(tok)

    def op(self, build, reads=(), writes=()):
        self.deps(reads, writes)
        if self.count >= SEM_LIMIT:
            self.epoch += 1
            self.sem = self.ctx.nc.alloc_semaphore("%s_s%d" % (self.name, self.epoch))
            self.count = 0
        self.count += 1
        r = _Rec()
        build(r)
        nm, a, k = r.call
        self.prog.append(lambda e, nm=nm, a=a, k=k, sem=self.sem: getattr(e, nm)(*a, **k).then_inc(sem, 1))
        tok = (self.sem, self.count, self)
        for t in reads:
            t.rd[self.name] = tok
        for t in writes:
            t.lw = tok
            t.rd = {}
        return tok


class DSem:
    def __init__(self, ctx, name):
        self.sem = ctx.nc.alloc_semaphore(name)
        self.count = 0
        self.name = name


class Ctx:
    def __init__(self, nc):
        self.nc = nc
        self.pe = Eng(self, "pe", is_pe=True)
        self.act = Eng(self, "act")
        self.dve = Eng(self, "dve")
        self.pool = Eng(self, "pool")
        self.sp = Eng(self, "sp")
        self.engs = [self.pe, self.act, self.dve, self.pool, self.sp]
        self.dsems = []

    def dsem(self, name=None):
        d = DSem(self, "%s_%d" % (name or "ds", len(self.dsems)))
        self.dsems.append(d)
        return d

    def dma(self, q, ds, out, in_, reads=(), writes=(), n=1, **kw):
        q.deps(reads, writes)
        pairs = list(zip(out, in_)) if isinstance(out, (list, tuple)) else [(out, in_)]
        for o, i in pairs:
            q.prog.append(lambda e, o=o, i=i, sem=ds.sem, kw=kw: e.dma_start(out=o, in_=i, **kw).then_inc(sem, 16))
            ds.count += 16
        tok = (ds.sem, ds.count, None)
        for t in reads:
            t.rd["dma_" + ds.name] = tok
        for t in writes:
            t.lw = tok
            t.rd = {}
        return tok

    def barrier(self):
        toks = [(E.sem, E.count, E) for E in self.engs if E.count > 0]
        toks += [(d.sem, d.count, None) for d in self.dsems if d.count > 0]
        for E in self.engs:
            for tok in toks:
                if tok[2] is E:
                    continue
                E._wait(tok)

    def emit(self):
        self.barrier()
        with self.nc.Block() as blk:
            def mk(E):
                def body(e):
                    for f in E.prog:
                        f(e)
                return body
            blk.tensor(mk(self.pe))
            blk.scalar(mk(self.act))
            blk.vector(mk(self.dve))
            blk.gpsimd(mk(self.pool))
            blk.sync(mk(self.sp))


class Scope:
    def __init__(self, c):
        self.c = c
        self.st = contextlib.ExitStack()

    _uid = [0]

    def sb(self, name, shape, dtype):
        Scope._uid[0] += 1
        t = self.st.enter_context(self.c.nc.sbuf_tensor("sb%d_%s" % (Scope._uid[0], name), list(shape), dtype))
        return Tk(t.ap() if hasattr(t, "ap") and callable(t.ap) else t, name)

    def close(self):
        self.c.barrier()
        self.st.close()


class Pipe:
    def __init__(self, depth=2):
        self.q = []
        self.depth = depth

    def push(self, fn):
        self.q.append(fn)
        while len(self.q) > self.depth:
            self.q.pop(0)()

    def flush(self):
        while self.q:
            self.q.pop(0)()


class WStream:
    def __init__(self, c, sc, nslots, width):
        self.c = c
        self.slots = [sc.sb("wslot%d" % i, [128, width], BF16) for i in range(nslots)]
        self.ds = [c.dsem("wsd%d" % i) for i in range(nslots)]
        self.queue = []
        self.inflight = []
        self.k = 0

    def push(self, items):
        self.queue.extend(items)
        self._fill()

    def _fill(self):
        while self.queue and len(self.inflight) < len(self.slots):
            it = self.queue.pop(0)
            i = self.k % len(self.slots)
            self.k += 1
            slot = self.slots[i]
            rds = []
            if isinstance(it, tuple):
                it, rds = it
            outs = [f(slot.ap) for f, _ in it]
            ins = [s for _, s in it]
            self.c.dma(self.c.pool, self.ds[i], outs, ins, reads=rds, writes=[slot], n=len(it))
            self.inflight.append(slot)

    def next(self):
        s = self.inflight.pop(0)
        return s

    def done(self):
        self._fill()


def host_consts(T=8192):
    cst = {}
    j = np.arange(128)[:, None]
    i = np.arange(128)[None, :]
    same = (j // 64) == (i // 64)
    cst["ltri"] = (np.where(same & (j <= i), -1.0 / 16, 0.0)).astype(np.float32)
    cst["mgt"] = (np.where(same & (j > i), -1.0 / 16, 0.0)).astype(np.float32)
    cst["mask01"] = (np.where(same & (j <= i), 1.0, 0.0)).astype(np.float32)
    cst["invc"] = np.tile((1.0 / (np.arange(16) + 1.0))[None, :], (128, 1)).astype(np.float32)
    cb = np.zeros((128, 256), np.float32)
    for p in range(128):
        cur = p // 64
        for rel in range(-128, 128):
            if rel > cur:
                v = -1.0
            elif rel == cur or rel == cur - 1:
                v = 1e4
            else:
                v = 0.0
            cb[p, 128 + rel] = v
    cst["cb"] = cb
    ew = np.zeros((128, 32, 128), np.float32)
    for p in range(128):
        r = p % 64
        for pt in range(32):
            for half in range(2):
                if r == 2 * pt + half:
                    ew[p, pt, half * 64:(half + 1) * 64] = 1.0
    cst["ew"] = ew.astype(ml_dtypes.bfloat16)
    k = np.arange(128)[:, None]
    q = np.arange(512)[None, :]
    cm = np.zeros((128, 4, 512), np.float32)
    wm = np.zeros((128, 4, 512), np.float32)
    for jj in range(4):
        cm[:, jj, :] = np.where(q < 128 * jj + k, -BIG, 0.0)
        wm[:, jj, :] = np.where(q >= 128 * jj + k, -BIG, 0.0)
    cst["cmask"] = cm.astype(ml_dtypes.bfloat16)
    cst["wmask"] = wm.astype(ml_dtypes.bfloat16)
    pm = np.zeros((128, 5, 512), np.float32)
    for v in range(5):
        off = 512 * v
        pm[:, v, :] = np.where(16 * k + 31 <= off + q, 0.0, -BIG)
    cst["pmask"] = pm.astype(ml_dtypes.bfloat16)
    qa = np.zeros((3, 16, 512), np.float32)
    for h in range(16):
        s = head_slope(h)
        s_hi = np.float32(np.float32(s).astype(ml_dtypes.bfloat16))
        s_lo = np.float32(s) - s_hi
        qa[0, h, :] = -s * np.arange(512)
        qa[1, h, :] = s_hi
        qa[2, h, :] = s_lo
    cst["qaug"] = qa.astype(ml_dtypes.bfloat16)
    ka = np.zeros((3, 128), np.float32)
    ka[0] = 1.0
    ka[1] = np.arange(128)
    ka[2] = np.arange(128)
    cst["kaug"] = ka.astype(ml_dtypes.bfloat16)
    kc = ka.copy()
    kc[1] *= 16
    kc[2] *= 16
    cst["kcaug"] = kc.astype(ml_dtypes.bfloat16)
    NCK = max(1, T // 2048)
    cst["kaugT"] = np.tile(ka[:, None, :], (1, 2, T // 128)).reshape(3, 2, T).astype(ml_dtypes.bfloat16)
    cst["kcaugT"] = np.tile(kc[:, None, :], (1, 2, NCK)).reshape(3, 2, NCK * 128).astype(ml_dtypes.bfloat16)
    n = np.arange(512)
    cs = n * 16
    ss = np.arange(128) * 64
    ov = np.minimum(cs[:, None] + 32, ss[None] + 64) - np.maximum(cs[:, None], ss[None])
    mcs = (np.clip(ov, 0, None) / 32.0).astype(np.float32)
    mcs[511] = 0.0
    cst["mcs"] = mcs.reshape(4, 128, 128).transpose(1, 0, 2).copy().astype(ml_dtypes.bfloat16)
    ident = np.eye(128, dtype=np.float32)
    cst["identf"] = ident
    cst["identb"] = ident.astype(ml_dtypes.bfloat16)
    cst["onesb"] = np.full((128, 128), 1.0 / D, np.float32).astype(ml_dtypes.bfloat16)
    return cst


CONST_DT = {"ltri": F32, "mgt": F32, "mask01": F32, "invc": F32, "cb": F32, "ew": BF16, "cmask": BF16,
            "wmask": BF16, "pmask": BF16, "qaug": BF16, "kaug": BF16, "kcaug": BF16, "kaugT": BF16, "kcaugT": BF16, "mcs": BF16,
            "identf": F32, "identb": BF16, "onesb": BF16}

IN_SHAPES = {
    "ffn_wg": [4, D, DFF], "ffn_wu": [4, D, DFF], "ffn_wd": [4, DFF, D],
    "gam": [128, 7 * 8], "a_w_in": [D, IN0], "w2a": [33, 256], "gnorm": [1, 128], "poolw": [4, 128, 128],
    "pscale": [128, 4], "a_w_out": [D, D], "c_w_in": [D, IN1], "pef": [128, 16],
    "cmpk_w1": [2048, 256], "cmpk_w2": [256, 64], "cmpv_w1": [2048, 256], "cmpv_w2": [256, 64], "c_w_out": [D, D],
}


def bias_plan(T):
    NT = T // TT
    plan = {}
    for h in range(16):
        for d in range(-3, 4 * NT + 1):
            plan[("s", h, d)] = len(plan)
    for h in range(16):
        for qt in range(NT):
            t0 = qt * TT
            nkc = ((t0 + 511 - 31) // 16) // 128 + 1
            for ktc in range(nkc):
                key = ("c", h, t0 - 2048 * ktc - 31)
                if key not in plan:
                    plan[key] = len(plan)
    return plan


def bias_table(T):
    plan = bias_plan(T)
    tab = np.zeros((128, len(plan)), np.float32)
    for (kind, h, v), i in plan.items():
        s_ = head_slope(h)
        tab[:, i] = -s_ * (128.0 * v if kind == "s" else float(v))
    return tab

def build_nc(T, stage="full"):
    NT = T // TT
    nc = bass.Bass("TRN2", target_bir_lowering=False)
    cst = host_consts(T)
    A = {}
    A["x"] = nc.dram_tensor("x", [T, D], F32, kind="ExternalInput").ap()
    for k, shp in IN_SHAPES.items():
        A[k] = nc.dram_tensor(k, shp, F32, kind="ExternalInput").ap()
    for k, v in cst.items():
        A[k] = nc.dram_tensor("c_" + k, list(v.shape), CONST_DT[k], kind="ExternalInput").ap()
    A["biasT"] = nc.dram_tensor("biasT", [128, len(bias_plan(T))], F32, kind="ExternalInput").ap()
    out = nc.dram_tensor("out", [T, D], F32, kind="ExternalOutput").ap()
    HS = Tk(nc.dram_tensor("hs", [128, 8, T], F32, kind="Internal").ap(), "hs")
    QS = Tk(nc.dram_tensor("qs", [64, 16, T], BF16, kind="Internal").ap(), "qs")
    KSs = {nm: Tk(nc.dram_tensor("s_" + nm, [64, 2, T], BF16, kind="Internal").ap(), nm) for nm in ("ks", "kw", "kc", "vc")}
    VSs = {nm: Tk(nc.dram_tensor("s_" + nm, [T, 2, 64], BF16, kind="Internal").ap(), nm) for nm in ("vs", "vw")}
    GS = Tk(nc.dram_tensor("gs", [T, 48], F32, kind="Internal").ap(), "gs")
    OS = Tk(nc.dram_tensor("os", [128, 8, T], BF16, kind="Internal").ap(), "os")

    c = Ctx(nc)
    P = [Tk(nc.alloc_psum_tensor("ps%d" % i, [128, 512], F32).ap(), "ps%d" % i) for i in range(8)]

    g_sc = Scope(c)
    onesb = g_sc.sb("onesb", [128, 128], BF16)
    identf = g_sc.sb("identf", [128, 128], F32)
    identb = g_sc.sb("identb", [128, 128], BF16)
    gam = g_sc.sb("gam", [128, 56], F32)
    epsb = g_sc.sb("epsb", [128, 1], F32)
    oneb = g_sc.sb("oneb", [128, 1], F32)
    c.dve.op(lambda e: e.memset(epsb[:], EPS), writes=[epsb])
    c.dve.op(lambda e: e.memset(oneb[:], 1.0), writes=[oneb])
    dsc = c.dsem("dconst")
    c.dma(c.sp, dsc, [onesb[:], identf[:], identb[:], gam[:]], [A["onesb"], A["identf"], A["identb"], A["gam"]],
          writes=[onesb, identf, identb, gam], n=4)

    def rmsnorm_fm(sc_bufs, hT, gi, hn):
        sq, lnb, rstd = sc_bufs
        for cc in range(8):
            s = sq[cc % 2]
            c.act.op(lambda e, cc=cc, s=s: e.activation(out=s[:], in_=hT[:, cc, :], func=AF.Square), reads=[hT], writes=[s])
            c.pe.op(lambda e, cc=cc, s=s: e.matmul(P[6][:, :], lhsT=onesb[:], rhs=s[:], start=(cc == 0), stop=(cc == 7)),
                    reads=[s, onesb], writes=[P[6]])
        c.act.op(lambda e: e.activation(out=lnb[:], in_=P[6][:, :], func=AF.Ln, bias=epsb[:], scale=1.0), reads=[P[6], epsb], writes=[lnb])
        c.act.op(lambda e: e.activation(out=rstd[:], in_=lnb[:], func=AF.Exp, scale=-0.5), reads=[lnb], writes=[rstd])
        for cc in range(8):
            c.dve.op(lambda e, cc=cc: e.scalar_tensor_tensor(out=hn[:, cc, :], in0=hT[:, cc, :], scalar=gam[:, gi * 8 + cc:gi * 8 + cc + 1],
                                                             op0=ALU.mult, in1=rstd[:], op1=ALU.mult),
                     reads=[hT, rstd, gam], writes=[hn])

    WB = nc.dram_tensor("wb", [4 * 30, 128, 2816], BF16, kind="Internal").ap()
    WBt = [[Tk(None, "wb%d_%d" % (fi, k)) for k in range(2)] for fi in range(4)]
    dconv = [[c.dsem("dconv%d_%d" % (fi, k)) for k in range(2)] for fi in range(4)]

    def convert_ffn(fi):
        n = 0
        for j in range(NF):
            for which, nm in ((0, "ffn_wg"), (1, "ffn_wu")):
                k = n % 2
                n += 1
                c.dma(c.pool, dconv[fi][k], WB[fi * 30 + j, :, which * 1024:(which + 1) * 1024].rearrange("p (c f) -> p c f", c=8),
                      A[nm][fi, :, j * 128:(j + 1) * 128].rearrange("(c p) f -> p c f", p=128), writes=[WBt[fi][k]])
        for cc in range(8):
            k = n % 2
            n += 1
            c.dma(c.pool, dconv[fi][k], WB[fi * 30 + NF + cc, :, 0:2816].rearrange("p (j f) -> p j f", j=NF),
                  A["ffn_wd"][fi, :, cc * 128:(cc + 1) * 128].rearrange("(j p) f -> p j f", p=128), writes=[WBt[fi][k]])

    def ffn_items(fi):
        items = []
        for j in range(NF):
            items.append(([(lambda s_: s_[:, 0:2048], WB[fi * 30 + j, :, 0:2048])], WBt[fi]))
        for cc in range(8):
            items.append(([(lambda s_: s_[:, 0:2816], WB[fi * 30 + NF + cc, :, 0:2816])], WBt[fi]))
        return items

    def ffn(ws, bufs, hT, gi):
        hn, aT, sgs, nb = bufs
        rmsnorm_fm(nb, hT, gi, hn)
        for j in range(NF):
            w = ws.next()
            pg, pu = P[j % 2], P[2 + j % 2]
            for cc in range(8):
                c.pe.op(lambda e, cc=cc, w=w, pg=pg: e.matmul(pg[:, :], lhsT=w[:, cc * 128:(cc + 1) * 128], rhs=hn[:, cc, :], start=(cc == 0), stop=(cc == 7)),
                        reads=[w, hn], writes=[pg])
            for cc in range(8):
                c.pe.op(lambda e, cc=cc, w=w, pu=pu: e.matmul(pu[:, :], lhsT=w[:, 1024 + cc * 128:1024 + (cc + 1) * 128], rhs=hn[:, cc, :], start=(cc == 0), stop=(cc == 7)),
                        reads=[w, hn], writes=[pu])
            ws.done()
            sg = sgs[j % 2]
            c.act.op(lambda e, pg=pg, sg=sg: e.activation(out=sg[:], in_=pg[:, :], func=AF.Silu), reads=[pg], writes=[sg])
            c.dve.op(lambda e, j=j, pu=pu, sg=sg: e.tensor_tensor(out=aT[:, j, :], in0=pu[:, :], in1=sg[:], op=ALU.mult), reads=[pu, sg], writes=[aT])
        for cc in range(8):
            w = ws.next()
            py = P[4 + cc % 2]
            for j in range(NF):
                c.pe.op(lambda e, j=j, w=w, py=py: e.matmul(py[:, :], lhsT=w[:, j * 128:(j + 1) * 128], rhs=aT[:, j, :], start=(j == 0), stop=(j == NF - 1)),
                        reads=[w, aT], writes=[py])
            ws.done()
            c.dve.op(lambda e, cc=cc, py=py: e.scalar_tensor_tensor(out=hT[:, cc, :], in0=py[:, :], scalar=0.5, op0=ALU.mult, in1=hT[:, cc, :], op1=ALU.add),
                     reads=[py, hT], writes=[hT])

    def load_resident(sc, name, src, ncols, ds=None):
        ds = c.dsem("d_" + name)
        t = sc.sb(name, [128, 8, ncols], BF16)
        step = 512
        outs, ins = [], []
        for c0 in range(0, ncols, step):
            c1 = min(ncols, c0 + step)
            outs.append(t[:, :, c0:c1])
            ins.append(src[:, c0:c1].rearrange("(c p) f -> p c f", p=128))
        c.dma(c.pool, ds, outs, ins, writes=[t], n=len(outs))
        return t

    def ffn_bufs(sc):
        hn = sc.sb("hn", [128, 8, TT], BF16)
        aT = sc.sb("aT", [128, NF, TT], BF16)
        sgs = [sc.sb("sg%d" % i, [128, TT], F32) for i in range(2)]
        sq = [sc.sb("sq%d" % i, [128, TT], BF16) for i in range(2)]
        lnb = sc.sb("lnb", [128, TT], F32)
        rstd = sc.sb("rstd", [128, TT], F32)
        return (hn, aT, sgs, (sq, lnb, rstd))

    def load_x_tile(t, xt, hT, ds):
        c.dma(c.sp, ds, xt[:], A["x"][t * TT:(t + 1) * TT, :].rearrange("(s p) d -> p s d", p=128), writes=[xt])
        for cc in range(8):
            pb = P[cc % 2]
            for s in range(4):
                c.pe.op(lambda e, cc=cc, s=s, pb=pb: e.transpose(out=pb[:, s * 128:(s + 1) * 128], in_=xt[:, s, cc * 128:(cc + 1) * 128], identity=identf[:]),
                        reads=[xt, identf], writes=[pb])
            c.act.op(lambda e, cc=cc, pb=pb: e.copy(out=hT[:, cc, :], in_=pb[:, :]), reads=[pb], writes=[hT])

    def store_out_tile(t, src, xt, ds):
        for s in range(4):
            for half in range(2):
                pb = P[(2 * s + half) % 2]
                for k in range(4):
                    cc = half * 4 + k
                    c.pe.op(lambda e, cc=cc, s=s, k=k, pb=pb: e.transpose(out=pb[:, k * 128:(k + 1) * 128], in_=src[:, cc, s * 128:(s + 1) * 128], identity=identf[:]),
                            reads=[src, identf], writes=[pb])
                c.act.op(lambda e, s=s, half=half, pb=pb: e.copy(out=xt[:, s, half * 512:(half + 1) * 512], in_=pb[:, :]), reads=[pb], writes=[xt])
        return c.dma(c.sp, ds, out[t * TT:(t + 1) * TT, :].rearrange("(s p) d -> p s d", p=128), xt[:], reads=[xt])

    convert_ffn(0)
    convert_ffn(2)
    sc = Scope(c)
    ws = WStream(c, sc, 3, 2816)
    fb = ffn_bufs(sc)
    hn = fb[0]
    hT = sc.sb("hT", [128, 8, TT], F32)
    xt = sc.sb("xt", [128, 4, D], F32)
    dres = c.dsem("dres")
    win0 = load_resident(sc, "win0", A["a_w_in"], IN0, dres)
    wout0 = load_resident(sc, "wout0", A["a_w_out"], D, dres)
    dx = c.dsem("dx")
    dhs = c.dsem("dhs")
    ltri = sc.sb("ltri", [128, 128], F32)
    mgt = sc.sb("mgt", [128, 128], F32)
    mask01 = sc.sb("mask01", [128, 128], F32)
    invc = sc.sb("invc", [128, 16], F32)
    w2a = sc.sb("w2a", [33, 256], F32)
    gnb = sc.sb("gnb", [128, 128], F32)
    poolw = sc.sb("poolw", [128, 4, 128], BF16)
    pscale = sc.sb("pscale", [128, 4], F32)
    dca = c.dsem("dca")
    c.dma(c.sp, dca, [ltri[:], mgt[:], mask01[:], invc[:], w2a[:], gnb[:], pscale[:]],
          [A["ltri"], A["mgt"], A["mask01"], A["invc"], A["w2a"], A["gnorm"].partition_broadcast(128), A["pscale"]],
          writes=[ltri, mgt, mask01, invc, w2a, gnb, pscale], n=7)
    c.dma(c.pool, c.dsem("dpoolw"), poolw[:], A["poolw"].rearrange("g c d -> c g d"), writes=[poolw])
    qT = sc.sb("qT", [128, 2, TT], F32)
    kT = sc.sb("kT", [128, 2, TT], F32)
    gra = sc.sb("gra", [33, TT], F32)
    c.dve.op(lambda e: e.memset(gra[:], 0.0), writes=[gra])
    c.dve.op(lambda e: e.memset(gra[32:33, :], 1.0), writes=[gra])
    lsp = sc.sb("lsp", [128, 256], F32)
    ebT = sc.sb("ebT", [128, 2, 128], F32)
    enbT = sc.sb("enbT", [128, 2, 128], F32)
    ekend = sc.sb("ekend", [128, 256], F32)
    qdec = sc.sb("qdec", [128, 2, 128], BF16)
    kinv = sc.sb("kinv", [128, 2, 128], BF16)
    kend = sc.sb("kend", [128, 256], BF16)
    vtok = sc.sb("vtok", [128, 512], BF16)
    sgT = sc.sb("sgT", [128, 4, TT], BF16)
    attm = [sc.sb("attm%d" % i, [128, 128], BF16) for i in range(2)]
    Sf = sc.sb("Sf", [128, 2, 128], F32)
    Sb = [sc.sb("Sb%d" % i, [128, 2, 128], BF16) for i in range(2)]
    c.dve.op(lambda e: e.memset(Sf[:], 0.0), writes=[Sf])
    c.dve.op(lambda e: e.memset(Sb[0][:], 0.0), writes=[Sb[0]])
    ss4 = sc.sb("ss4", [128, 4], F32)
    junk = sc.sb("junk", [128, 128], F32)
    on_t = sc.sb("on_t", [128, 512], F32)
    oT = sc.sb("oT", [128, 8, TT], BF16)
    pT = sc.sb("pT", [128, 4, 16 + TT], F32)
    sA = sc.sb("sA", [128, 4, 16 + TT], F32)
    sB = sc.sb("sB", [128, 4, 16 + TT], F32)
    pooled = sc.sb("pooled", [128, 4, TT], BF16)
    c.dve.op(lambda e: e.memset(pT[:], 0.0), writes=[pT])

    import os
    CUT = float(os.environ.get('CUT', '99'))

    def body_mix(t):
        if CUT >= 11: ws.push(ffn_items(2))
        rmsnorm_fm(fb[3], hT, 2, hn)
        for m in range(2):
            for which, dst, scl in ((0, qT, 0.125), (1, kT, 1.0)):
                pb = P[(2 * m + which) % 2]
                col = which * 256 + m * 128
                for cc in range(8):
                    c.pe.op(lambda e, cc=cc, pb=pb, col=col: e.matmul(pb[:, :], lhsT=win0[:, cc, col:col + 128], rhs=hn[:, cc, :], start=(cc == 0), stop=(cc == 7)),
                            reads=[win0, hn], writes=[pb])
                c.act.op(lambda e, dst=dst, m=m, pb=pb, scl=scl: e.activation(out=dst[:, m, :], in_=pb[:, :], func=AF.Copy, scale=scl), reads=[pb], writes=[dst])
        if CUT < 1: return
        for cc in range(8):
            c.pe.op(lambda e, cc=cc: e.matmul(P[2][0:16, :], lhsT=win0[:, cc, 1536:1552], rhs=hn[:, cc, :], start=(cc == 0), stop=(cc == 7)),
                    reads=[win0, hn], writes=[P[2]])
        c.act.op(lambda e: e.copy(out=gra[0:16, :], in_=P[2][0:16, :]), reads=[P[2]], writes=[gra])
        if CUT < 2: return
        for g in range(4):
            pb = P[3]
            for cc in range(8):
                c.pe.op(lambda e, cc=cc, g=g, pb=pb: e.matmul(pb[:, :], lhsT=win0[:, cc, 1552 + g * 128:1552 + (g + 1) * 128], rhs=hn[:, cc, :], start=(cc == 0), stop=(cc == 7)),
                        reads=[win0, hn], writes=[pb])
            c.act.op(lambda e, g=g, pb=pb: e.copy(out=pT[:, g, 16:16 + TT], in_=pb[:, :]), reads=[pb], writes=[pT])
        if CUT < 3: return
        W = 16 + TT
        c.pool.op(lambda e: e.tensor_tensor(out=sA[:, 0:4, 1:W], in0=pT[:, 0:4, 1:W], in1=pT[:, 0:4, 0:W - 1], op=ALU.add), reads=[pT], writes=[sA])
        c.pool.op(lambda e: e.tensor_tensor(out=sB[:, 1:4, 3:W], in0=sA[:, 1:4, 3:W], in1=sA[:, 1:4, 1:W - 2], op=ALU.add), reads=[sA], writes=[sB])
        c.pool.op(lambda e: e.tensor_tensor(out=sA[:, 2:4, 7:W], in0=sB[:, 2:4, 7:W], in1=sB[:, 2:4, 3:W - 4], op=ALU.add), reads=[sB], writes=[sA])
        c.pool.op(lambda e: e.tensor_tensor(out=sB[:, 3:4, 15:W], in0=sA[:, 3:4, 15:W], in1=sA[:, 3:4, 7:W - 8], op=ALU.add), reads=[sA], writes=[sB])
        for g in range(4):
            src = sA if g % 2 == 0 else sB
            wdw = 2 ** (g + 1)
            c.dve.op(lambda e, g=g, src=src, wdw=wdw: e.scalar_tensor_tensor(out=pooled[:, g, :], in0=src[:, g, 16:W], scalar=1.0 / wdw, op0=ALU.mult,
                                                                             in1=pT[:, g, 16:W], op1=ALU.subtract),
                     reads=[src, pT], writes=[pooled])
            if t == 0:
                n = wdw - 1
                c.dve.op(lambda e, g=g, src=src, n=n: e.tensor_tensor(out=junk[:, 0:n], in0=src[:, g, 16:16 + n], in1=invc[:, 0:n], op=ALU.mult),
                         reads=[src, invc], writes=[junk])
                c.dve.op(lambda e, g=g, n=n: e.tensor_tensor(out=pooled[:, g, 0:n], in0=junk[:, 0:n], in1=pT[:, g, 16:16 + n], op=ALU.subtract),
                         reads=[junk, pT], writes=[pooled])
        c.pool.op(lambda e: e.tensor_copy(out=sA[:, :, 0:16], in_=pT[:, :, TT:TT + 16]), reads=[pT], writes=[sA])
        c.pool.op(lambda e: e.tensor_copy(out=pT[:, :, 0:16], in_=sA[:, :, 0:16]), reads=[sA], writes=[pT])
        for g in range(4):
            pb = P[3]
            c.pe.op(lambda e, g=g, pb=pb: e.matmul(pb[:, :], lhsT=poolw[:, g, :], rhs=pooled[:, g, :], start=True, stop=True), reads=[poolw, pooled], writes=[pb])
            c.act.op(lambda e, g=g, pb=pb: e.activation(out=oT[:, 4 + g, :], in_=pb[:, :], func=AF.Identity, scale=pscale[:, g:g + 1]), reads=[pb, pscale], writes=[oT])
        for h in range(4):
            pb = P[4 + h % 2]
            for cc in range(8):
                c.pe.op(lambda e, cc=cc, h=h, pb=pb: e.matmul(pb[:, :], lhsT=win0[:, cc, 1024 + h * 128:1024 + (h + 1) * 128], rhs=hn[:, cc, :], start=(cc == 0), stop=(cc == 7)),
                        reads=[win0, hn], writes=[pb])
            c.act.op(lambda e, h=h, pb=pb: e.activation(out=sgT[:, h, :], in_=pb[:, :], func=AF.Silu), reads=[pb], writes=[sgT])
        if CUT < 4: return
        for s in range(int(os.environ.get('NSUB', '4'))):
            ts = slice(s * 128, (s + 1) * 128)
            c.pe.op(lambda e, ts=ts: e.matmul(P[0][:, 0:256], lhsT=gra[0:33, ts], rhs=w2a[0:33, :], start=True, stop=True), reads=[gra, w2a], writes=[P[0]])
            c.act.op(lambda e: e.activation(out=lsp[:], in_=P[0][:, 0:256], func=AF.Exp, scale=-1.0), reads=[P[0]], writes=[lsp])
            c.act.op(lambda e: e.activation(out=lsp[:], in_=lsp[:], func=AF.Ln, bias=oneb[:], scale=1.0), reads=[lsp, oneb], writes=[lsp])
            if CUT < 5: continue
            for m in range(2):
                c.pe.op(lambda e, m=m: e.matmul(P[1][:, m * 128:(m + 1) * 128], lhsT=lsp[:, m * 128:(m + 1) * 128], rhs=ltri[:], start=True, stop=True),
                        reads=[lsp, ltri], writes=[P[1]])
            c.pe.op(lambda e: e.matmul(P[0][:, 256:512], lhsT=mgt[:], rhs=lsp[:], start=True, stop=True), reads=[lsp, mgt], writes=[P[0]])
            c.act.op(lambda e: e.activation(out=ebT[:].rearrange("p m i -> p (m i)"), in_=P[1][:, 0:256], func=AF.Exp), reads=[P[1]], writes=[ebT])
            c.act.op(lambda e: e.activation(out=enbT[:].rearrange("p m i -> p (m i)"), in_=P[1][:, 0:256], func=AF.Exp, scale=-1.0), reads=[P[1]], writes=[enbT])
            c.act.op(lambda e: e.activation(out=ekend[:], in_=P[0][:, 256:512], func=AF.Exp), reads=[P[0]], writes=[ekend])
            c.dve.op(lambda e, ts=ts: e.tensor_tensor(out=qdec[:], in0=qT[:, :, ts], in1=ebT[:], op=ALU.mult), reads=[qT, ebT], writes=[qdec])
            c.dve.op(lambda e, ts=ts: e.tensor_tensor(out=kinv[:], in0=kT[:, :, ts], in1=enbT[:], op=ALU.mult), reads=[kT, enbT], writes=[kinv])
            if CUT < 6: continue
            for cc in range(8):
                c.pe.op(lambda e, cc=cc, ts=ts: e.matmul(P[2][:, 0:256], lhsT=hn[:, cc, ts], rhs=win0[:, cc, 256:512], start=(cc == 0), stop=(cc == 7)),
                        reads=[hn, win0], writes=[P[2]])
            c.dve.op(lambda e: e.tensor_tensor(out=kend[:], in0=P[2][:, 0:256], in1=ekend[:], op=ALU.mult), reads=[P[2], ekend], writes=[kend])
            if CUT < 6.3: continue
            for cc in range(8):
                c.pe.op(lambda e, cc=cc, ts=ts: e.matmul(P[3][:, :], lhsT=hn[:, cc, ts], rhs=win0[:, cc, 512:1024], start=(cc == 0), stop=(cc == 7)),
                        reads=[hn, win0], writes=[P[3]])
            c.act.op(lambda e: e.copy(out=vtok[:], in_=P[3][:, :]), reads=[P[3]], writes=[vtok])
            DSB = (P[5], P[1])
            for ch in range(2):
                tb = slice(ch * 64, (ch + 1) * 64)
                for h in range(4):
                    m, po = h // 2, (h % 2) * 64
                    c.pe.op(lambda e, ch=ch, tb=tb, h=h, m=m, po=po: e.matmul(DSB[ch][po:po + 64, 256 + m * 128:256 + (m + 1) * 128],
                                                                             lhsT=kend[tb, h * 64:(h + 1) * 64], rhs=vtok[tb, h * 128:(h + 1) * 128], start=True, stop=True),
                            reads=[kend, vtok], writes=[DSB[ch]])
            if CUT < 8: continue
            for ch in range(2):
                for m in range(2):
                    c.dve.op(lambda e, ch=ch, m=m: e.scalar_tensor_tensor(out=Sf[:, m, :], in0=Sf[:, m, :], scalar=ebT[:, m, ch * 64 + 63:ch * 64 + 64], op0=ALU.mult,
                                                                          in1=DSB[ch][:, 256 + m * 128:256 + (m + 1) * 128], op1=ALU.add),
                             reads=[Sf, ebT, DSB[ch]], writes=[Sf])
                if ch == 0:
                    c.act.op(lambda e: e.copy(out=Sb[1][:], in_=Sf[:]), reads=[Sf], writes=[Sb[1]])
            if CUT < 9: continue
            for h in range(4):
                m, po = h // 2, (h % 2) * 64
                pa = P[6 + h % 2] if False else P[6]
                am = attm[h % 2]
                c.pe.op(lambda e, m=m, po=po, pa=pa: e.matmul(pa[:, 0:128], lhsT=kinv[po:po + 64, m, :], rhs=qdec[po:po + 64, m, :], start=True, stop=True),
                        reads=[kinv, qdec], writes=[pa])
                c.dve.op(lambda e, pa=pa, am=am: e.tensor_tensor(out=am[:], in0=pa[:, 0:128], in1=mask01[:], op=ALU.mult), reads=[pa, mask01], writes=[am])
                hc = slice(h * 128, (h + 1) * 128)
                c.pe.op(lambda e, am=am, hc=hc: e.matmul(P[7][:, hc], lhsT=am[:], rhs=vtok[:, hc], start=True, stop=False, skip_group_check=True), reads=[am, vtok], writes=[P[7]])
                c.pe.op(lambda e, m=m, po=po, hc=hc: e.matmul(P[7][0:64, hc], lhsT=qdec[po:po + 64, m, 0:64], rhs=Sb[0][po:po + 64, m, :], start=False, stop=True, skip_group_check=True),
                        reads=[qdec, Sb[0]], writes=[P[7]])
                c.pe.op(lambda e, m=m, po=po, hc=hc: e.matmul(P[7][64:128, hc], lhsT=qdec[po:po + 64, m, 64:128], rhs=Sb[1][po:po + 64, m, :], start=False, stop=True, skip_group_check=True),
                        reads=[qdec, Sb[1]], writes=[P[7]])
            c.act.op(lambda e: e.copy(out=Sb[0][:], in_=Sf[:]), reads=[Sf], writes=[Sb[0]])
            if CUT < 10: continue
            for h in range(4):
                hc = slice(h * 128, (h + 1) * 128)
                c.act.op(lambda e, h=h, hc=hc: e.activation(out=junk[:], in_=P[7][:, hc], func=AF.Square, accum_out=ss4[:, h:h + 1]), reads=[P[7]], writes=[junk, ss4])
            c.act.op(lambda e: e.activation(out=ss4[:], in_=ss4[:], func=AF.Ln, bias=epsb[:], scale=1.0 / 128), reads=[ss4, epsb], writes=[ss4])
            c.act.op(lambda e: e.activation(out=ss4[:], in_=ss4[:], func=AF.Exp, scale=-0.5), reads=[ss4], writes=[ss4])
            for h in range(4):
                hc = slice(h * 128, (h + 1) * 128)
                c.dve.op(lambda e, h=h, hc=hc: e.scalar_tensor_tensor(out=on_t[:, hc], in0=P[7][:, hc], scalar=ss4[:, h:h + 1], op0=ALU.mult, in1=gnb[:], op1=ALU.mult),
                         reads=[P[7], ss4, gnb], writes=[on_t])
            if CUT < 10: continue
            for h in range(4):
                c.pe.op(lambda e, h=h: e.transpose(out=P[4][:, h * 128:(h + 1) * 128], in_=on_t[:, h * 128:(h + 1) * 128], identity=identf[:]),
                        reads=[on_t, identf], writes=[P[4]])
            c.dve.op(lambda e, ts=ts: e.tensor_tensor(out=oT[:, 0:4, ts], in0=P[4][:, :].rearrange("p (h i) -> p h i", h=4), in1=sgT[:, :, ts], op=ALU.mult),
                     reads=[P[4], sgT], writes=[oT])
        if CUT < 10: return
        for cc in range(8):
            pb = P[cc % 2]
            for k in range(8):
                c.pe.op(lambda e, cc=cc, k=k, pb=pb: e.matmul(pb[:, :], lhsT=wout0[:, k, cc * 128:(cc + 1) * 128], rhs=oT[:, k, :], start=(k == 0), stop=(k == 7)),
                        reads=[wout0, oT], writes=[pb])
            c.dve.op(lambda e, cc=cc, pb=pb: e.tensor_tensor(out=hT[:, cc, :], in0=pb[:, :], in1=hT[:, cc, :], op=ALU.add), reads=[pb, hT], writes=[hT])

        if CUT >= 11: ffn(ws, fb, hT, 4)

    for l0 in range(1):
        for t in range(NT):
            if stage != "X":
                ws.push(ffn_items(0))
            load_x_tile(t, xt, hT, dx)
            if stage == "X":
                store_out_tile(t, hT, xt, dx)
                continue
            ffn(ws, fb, hT, 0)
            if stage == "F":
                store_out_tile(t, hT, xt, dx)
                continue
            if stage == "A":
                body_mix(t)
                store_out_tile(t, hT, xt, dx)
                continue
            body_mix(t)
            c.dma(c.sp, dhs, HS[:, :, t * TT:(t + 1) * TT], hT[:], reads=[hT], writes=[HS])
            if t == 0:
                convert_ffn(1)
                convert_ffn(3)
    sc.close()
    if stage in ("A", "X", "F"):
        c.emit()
        return nc
    NKT = T // 128
    NCOL = T // 16
    NCK = max(1, T // 2048)
    bplan = bias_plan(T)
    biasT = g_sc.sb("biasT", [128, len(bplan)], F32)
    c.dma(c.sp, c.dsem("dbias"), biasT[:], A["biasT"], writes=[biasT])

    def bcol(key):
        i = bplan[key]
        return biasT[:, i:i + 1]

    sc = Scope(c)
    ws = WStream(c, sc, 3, 2816)
    fb = ffn_bufs(sc)
    hn = fb[0]
    hT = sc.sb("hT", [128, 8, TT], F32)
    xt = sc.sb("xt", [128, 4, D], F32)
    dx = c.dsem("dxB")
    dhl = c.dsem("dhlB")
    dhs2 = c.dsem("dhsB")
    win1 = load_resident(sc, "win1", A["c_w_in"], IN1)
    qst = sc.sb("qst", [64, 16, TT], BF16)
    kst = {nm: sc.sb("kst_" + nm, [64, 2, TT], BF16) for nm in ("kc", "vc", "ks", "kw")}
    vfm = sc.sb("vfm", [128, TT], BF16)
    vst = {nm: sc.sb("vst_" + nm, [128, 4, 128], BF16) for nm in ("vs", "vw")}
    gfm = sc.sb("gfm", [48, TT], F32)
    gst = sc.sb("gst", [128, 4, 48], F32)
    dst_ = {nm: c.dsem("dst_" + nm) for nm in ("q", "kc", "vc", "ks", "kw", "vs", "vw", "g")}
    KCOL = {"kc": 1024, "vc": 1152, "ks": 1280, "vs": 1408, "kw": 1536, "vw": 1664}
    for t in range(NT):
        tsl = slice(t * TT, (t + 1) * TT)
        ws.push(ffn_items(1))
        c.dma(c.sp, dhl, hT[:], HS[:, :, tsl], reads=[HS], writes=[hT])
        ffn(ws, fb, hT, 1)
        if stage == "B":
            store_out_tile(t, hT, xt, dx)
            continue
        c.dma(c.sp, dhs2, HS[:, :, tsl], hT[:], reads=[hT], writes=[HS])
        rmsnorm_fm(fb[3], hT, 3, hn)
        for m in range(8):
            pb = P[m % 2]
            for cc in range(8):
                c.pe.op(lambda e, cc=cc, m=m, pb=pb: e.matmul(pb[:, :], lhsT=win1[:, cc, m * 128:(m + 1) * 128], rhs=hn[:, cc, :], start=(cc == 0), stop=(cc == 7)),
                        reads=[win1, hn], writes=[pb])
            c.dve.op(lambda e, m=m, pb=pb: e.tensor_scalar(out=qst[:, 2 * m, :], in0=pb[0:64, :], scalar1=0.125, scalar2=None, op0=ALU.mult), reads=[pb], writes=[qst])
            c.dve.op(lambda e, m=m, pb=pb: e.tensor_scalar(out=qst[:, 2 * m + 1, :], in0=pb[64:128, :], scalar1=0.125, scalar2=None, op0=ALU.mult), reads=[pb], writes=[qst])
        c.dma(c.sp, dst_["q"], QS[:, :, tsl], qst[:], reads=[qst], writes=[QS])
        for i, nm in enumerate(("kc", "vc", "ks", "kw")):
            pb = P[2 + i % 2]
            col = KCOL[nm]
            for cc in range(8):
                c.pe.op(lambda e, cc=cc, col=col, pb=pb: e.matmul(pb[:, :], lhsT=win1[:, cc, col:col + 128], rhs=hn[:, cc, :], start=(cc == 0), stop=(cc == 7)),
                        reads=[win1, hn], writes=[pb])
            c.dve.op(lambda e, nm=nm, pb=pb: e.tensor_copy(out=kst[nm][:, 0, :], in_=pb[0:64, :]), reads=[pb], writes=[kst[nm]])
            c.dve.op(lambda e, nm=nm, pb=pb: e.tensor_copy(out=kst[nm][:, 1, :], in_=pb[64:128, :]), reads=[pb], writes=[kst[nm]])
            c.dma(c.sp, dst_[nm], KSs[nm][:, :, tsl], kst[nm][:], reads=[kst[nm]], writes=[KSs[nm]])
        for i, nm in enumerate(("vs", "vw")):
            pb = P[4 + i]
            col = KCOL[nm]
            for cc in range(8):
                c.pe.op(lambda e, cc=cc, col=col, pb=pb: e.matmul(pb[:, :], lhsT=win1[:, cc, col:col + 128], rhs=hn[:, cc, :], start=(cc == 0), stop=(cc == 7)),
                        reads=[win1, hn], writes=[pb])
            c.act.op(lambda e, pb=pb: e.copy(out=vfm[:], in_=pb[:, :]), reads=[pb], writes=[vfm])
            ptb = P[7].ap.bitcast(BF16)
            for s4 in range(4):
                c.pe.op(lambda e, s4=s4, ptb=ptb: e.transpose(out=ptb[:, s4 * 128:(s4 + 1) * 128], in_=vfm[:, s4 * 128:(s4 + 1) * 128], identity=identb[:]),
                        reads=[vfm, identb], writes=[P[7]])
            c.dve.op(lambda e, nm=nm, ptb=ptb: e.tensor_copy(out=vst[nm][:].rearrange("p s f -> p (s f)"), in_=ptb[:, 0:512]), reads=[P[7]], writes=[vst[nm]])
            c.dma(c.sp, dst_[nm], VSs[nm][tsl, :, :].rearrange("(s p) g d -> p s (g d)", p=128), vst[nm][:], reads=[vst[nm]], writes=[VSs[nm]])
        for cc in range(8):
            c.pe.op(lambda e, cc=cc: e.matmul(P[6][0:48, :], lhsT=win1[:, cc, 1792:1840], rhs=hn[:, cc, :], start=(cc == 0), stop=(cc == 7)),
                    reads=[win1, hn], writes=[P[6]])
        c.act.op(lambda e: e.activation(out=gfm[:], in_=P[6][0:48, :], func=AF.Sigmoid), reads=[P[6]], writes=[gfm])
        for s4 in range(4):
            c.pe.op(lambda e, s4=s4: e.transpose(out=P[7][:, 256 + s4 * 48:256 + (s4 + 1) * 48], in_=gfm[0:48, s4 * 128:(s4 + 1) * 128], identity=identf[0:48, 0:48]),
                    reads=[gfm, identf], writes=[P[7]])
        c.act.op(lambda e: e.copy(out=gst[:].rearrange("p s f -> p (s f)"), in_=P[7][:, 256:256 + 192]), reads=[P[7]], writes=[gst])
        c.dma(c.sp, dst_["g"], GS[tsl, :].rearrange("(s p) f -> p s f", p=128), gst[:], reads=[gst], writes=[GS])
    sc.close()
    if stage == "B":
        c.emit()
        return nc

    pd = Scope(c)
    KC = pd.sb("KC", [67, 2, NCK * 128], BF16)
    VCM = pd.sb("VCM", [128, NCK, 2, 65], BF16)
    c.dve.op(lambda e: e.memset(KC[:], 0.0), writes=[KC])
    c.dve.op(lambda e: e.memset(VCM[:], 0.0), writes=[VCM])
    c.dve.op(lambda e: e.memset(VCM[:, :, :, 64:65], 1.0), writes=[VCM])
    c.dma(c.sp, c.dsem("dkcaug"), KC[64:67, :, :], A["kcaugT"], writes=[KC])
    sc = Scope(c)
    stk = sc.sb("stk", [128, T + 16], BF16)
    w1 = sc.sb("w1", [128, 16, 256], BF16)
    w2 = sc.sb("w2", [128, 2, 64], BF16)
    pef = sc.sb("pef", [128, 16], BF16)
    pebias = sc.sb("pebias", [128, 2], F32)
    xg = sc.sb("xg", [128, 512], F32)
    x2 = sc.sb("x2", [128, 512], F32)
    sgm = sc.sb("sgm", [128, 512], F32)
    hg = sc.sb("hg", [128, 2, 512], BF16)
    c.dma(c.pool, c.dsem("dpef"), pef[:], A["pef"], writes=[pef])
    dstk = c.dsem("dstk")
    dw1 = c.dsem("dw1")
    for zi, (znm, w1n, w2n) in enumerate((("kc", "cmpk_w1", "cmpk_w2"), ("vc", "cmpv_w1", "cmpv_w2"))):
        c.dma(c.pool, dw1, [w1[:], w2[:]], [A[w1n].rearrange("(m p) c -> p m c", p=128), A[w2n].rearrange("(k p) d -> p k d", p=128)], writes=[w1, w2])
        for cc in range(2):
            for m in range(16):
                c.pe.op(lambda e, cc=cc, m=m: e.matmul(P[2][:, cc:cc + 1], lhsT=w1[:, m, cc * 128:(cc + 1) * 128], rhs=pef[:, m:m + 1], start=(m == 0), stop=(m == 15)),
                        reads=[w1, pef], writes=[P[2]])
        c.act.op(lambda e: e.copy(out=pebias[:], in_=P[2][:, 0:2]), reads=[P[2]], writes=[pebias])
        for g in range(2):
            c.dve.op(lambda e: e.memset(stk[:], 0.0), writes=[stk])
            c.dma(c.sp, dstk, [stk[0:64, 0:T], stk[64:128, 0:T - 1]], [KSs[znm][:, g, 0:T], KSs[znm][:, g, 1:T]], reads=[KSs[znm]], writes=[stk])
            for cc in range(2):
                for m in range(16):
                    c.pe.op(lambda e, cc=cc, m=m: e.matmul(P[cc][:, 0:NCOL], lhsT=w1[:, m, cc * 128:(cc + 1) * 128], rhs=stk[:, 2 * m:2 * m + 16 * (NCOL - 1) + 1:16], start=(m == 0), stop=(m == 15)),
                            reads=[w1, stk], writes=[P[cc]])
                c.act.op(lambda e, cc=cc: e.activation(out=xg[:, 0:NCOL], in_=P[cc][:, 0:NCOL], func=AF.Identity, bias=pebias[:, cc:cc + 1], scale=1.0), reads=[P[cc], pebias], writes=[xg])
                c.dve.op(lambda e: e.tensor_tensor(out=x2[:, 0:NCOL], in0=xg[:, 0:NCOL], in1=xg[:, 0:NCOL], op=ALU.mult), reads=[xg], writes=[x2])
                c.dve.op(lambda e: e.tensor_scalar(out=x2[:, 0:NCOL], in0=x2[:, 0:NCOL], scalar1=0.044715, scalar2=1.0, op0=ALU.mult, op1=ALU.add), reads=[x2], writes=[x2])
                c.dve.op(lambda e: e.tensor_tensor(out=x2[:, 0:NCOL], in0=x2[:, 0:NCOL], in1=xg[:, 0:NCOL], op=ALU.mult), reads=[x2, xg], writes=[x2])
                c.act.op(lambda e: e.activation(out=sgm[:, 0:NCOL], in_=x2[:, 0:NCOL], func=AF.Sigmoid, scale=1.5957691216057308), reads=[x2], writes=[sgm])
                c.dve.op(lambda e, cc=cc: e.tensor_tensor(out=hg[:, cc, 0:NCOL], in0=xg[:, 0:NCOL], in1=sgm[:, 0:NCOL], op=ALU.mult), reads=[xg, sgm], writes=[hg])
            if zi == 0:
                for cc in range(2):
                    c.pe.op(lambda e, cc=cc: e.matmul(P[3][0:64, 0:NCOL], lhsT=w2[:, cc, :], rhs=hg[:, cc, 0:NCOL], start=(cc == 0), stop=(cc == 1)), reads=[w2, hg], writes=[P[3]])
                c.act.op(lambda e, g=g: e.copy(out=KC[0:64, g, 0:NCOL], in_=P[3][0:64, 0:NCOL]), reads=[P[3]], writes=[KC])
            else:
                for nb in range((NCOL + 127) // 128):
                    cols = min(128, NCOL - nb * 128)
                    for cc in range(2):
                        c.pe.op(lambda e, cc=cc, nb=nb, cols=cols: e.matmul(P[4][0:cols, 0:64], lhsT=hg[:, cc, nb * 128:nb * 128 + cols], rhs=w2[:, cc, :], start=(cc == 0), stop=(cc == 1)),
                                reads=[w2, hg], writes=[P[4]])
                    c.act.op(lambda e, g=g, nb=nb, cols=cols: e.copy(out=VCM[0:cols, nb, g, 0:64], in_=P[4][0:cols, 0:64]), reads=[P[4]], writes=[VCM])
    sc.close()

    sc = Scope(c)
    KS = sc.sb("KS", [67, 2, T], BF16)
    VS = sc.sb("VS", [128, NKT, 2, 65], BF16)
    c.dve.op(lambda e: e.memset(VS[:, :, :, 64:65], 1.0), writes=[VS])
    c.dma(c.sp, c.dsem("dKS"), [KS[0:64, :, :], KS[64:67, :, :]], [KSs["ks"][:, :, :], A["kaugT"]], reads=[KSs["ks"]], writes=[KS])
    dVS = c.dsem("dVS")
    step = 16
    c.dma(c.sp, dVS, [VS[:, k0:min(NKT, k0 + step), g, 0:64] for k0 in range(0, NKT, step) for g in range(2)],
          [VSs["vs"][k0 * 128:min(NKT, k0 + step) * 128, g, :].rearrange("(k p) d -> p k d", p=128) for k0 in range(0, NKT, step) for g in range(2)],
          reads=[VSs["vs"]], writes=[VS])
    QA = [sc.sb("QA%d" % i, [67, 16, TT], BF16) for i in range(2)]
    KWt = [sc.sb("KWt%d" % i, [67, 2, 1024], BF16) for i in range(2)]
    VWt = [sc.sb("VWt%d" % i, [128, 8, 2, 65], BF16) for i in range(2)]
    for i in range(2):
        c.dve.op(lambda e, i=i: e.memset(VWt[i][:, :, :, 64:65], 1.0), writes=[VWt[i]])
        c.dma(c.sp, c.dsem("dqaug%d" % i), [QA[i][64:67, :, :], KWt[i][64:67, :, :]], [A["qaug"], A["kaugT"][:, :, 0:1024]], writes=[QA[i], KWt[i]])
    dQA = [c.dsem("dQA%d" % i) for i in range(2)]
    dKW = [c.dsem("dKW%d" % i) for i in range(2)]
    dVW = [c.dsem("dVW%d" % i) for i in range(2)]
    gt = sc.sb("gt", [128, 4, 48], F32)
    dgt = c.dsem("dgt")
    oacc = sc.sb("oacc", [128, 4, D], F32)
    impacc = sc.sb("impacc", [128, 4, 128], F32)
    impm = sc.sb("impm", [128, 128], F32)
    rep = sc.sb("rep", [128, 128], F32)
    mx8 = sc.sb("mx8", [128, 16], F32)
    thr = sc.sb("thr", [128, 1], F32)
    selB = sc.sb("selB", [128, 128], BF16)
    selT = sc.sb("selT", [128, TT], BF16)
    PT = [sc.sb("PT%d" % i, [128, TT], BF16) for i in range(5)]
    rs = sc.sb("rs", [128, 4], F32)
    fac = sc.sb("fac", [128, 4], F32)
    cmask = sc.sb("cmask", [128, 4, TT], BF16)
    wmask = sc.sb("wmask", [128, 4, TT], BF16)
    pmask = sc.sb("pmask", [128, 5, TT], BF16)
    ew = sc.sb("ew", [128, 32, 128], BF16)
    mcs = sc.sb("mcs", [128, 4, 128], BF16)
    cb = sc.sb("cb", [128, 256], F32)
    oTn = sc.sb("oTn", [128, 8, TT], BF16)
    dOS = c.dsem("dOS")
    c.dma(c.sp, c.dsem("dmasks"), [cmask[:], wmask[:], pmask[:], ew[:], mcs[:], cb[:]], [A["cmask"], A["wmask"], A["pmask"], A["ew"], A["mcs"], A["cb"]],
          writes=[cmask, wmask, pmask, ew, mcs, cb], n=6)
    pti = [0]
    psi = [0]
    poi = [0]
    pipe = Pipe(2)
    sbanks = [[P[0], P[1], P[2]]]

    def score_tile(lhs_k, qa_h, extra, bias_ap, cr=(0, TT)):
        c0, c1 = cr
        ps = sbanks[0][psi[0] % len(sbanks[0])]
        psi[0] += 1
        n_mm = 1 + len(extra)
        kt_tk, k_ap = lhs_k
        qa_tk, q_ap = qa_h
        c.pe.op(lambda e: e.matmul(ps[:, c0:c1], lhsT=k_ap, rhs=q_ap[:, c0:c1], start=True, stop=(n_mm == 1)), reads=[kt_tk, qa_tk], writes=[ps])
        for i, (l_ap, r_ap, rds) in enumerate(extra):
            c.pe.op(lambda e, l_ap=l_ap, r_ap=r_ap, last=(i == len(extra) - 1): e.matmul(ps[:, c0:c1], lhsT=l_ap, rhs=r_ap[:, c0:c1], start=False, stop=last), reads=rds, writes=[ps])
        pt = PT[pti[0] % 5]
        pti[0] += 1
        c.act.op(lambda e: e.activation(out=pt[:, c0:c1], in_=ps[:, c0:c1], func=AF.Exp, bias=bias_ap, scale=1.0), reads=[ps, biasT], writes=[pt])
        return pt

    def finish_head(po, h, br, first):
        c.dve.op(lambda e, po=po: e.tensor_scalar(out=rs[:], in0=po[:, 0:260].rearrange("p (s f) -> p s f", f=65)[:, :, 64], scalar1=1e-30, scalar2=None, op0=ALU.add), reads=[po], writes=[rs])
        c.dve.op(lambda e: e.reciprocal(out=rs[:], in_=rs[:]), reads=[rs], writes=[rs])
        c.dve.op(lambda e, h=h, br=br: e.tensor_tensor(out=fac[:], in0=rs[:], in1=gt[:, :, 3 * h + br], op=ALU.mult), reads=[rs, gt], writes=[fac])
        for s4 in range(4):
            if first:
                c.dve.op(lambda e, po=po, s4=s4, h=h: e.tensor_scalar(out=oacc[:, s4, h * 64:(h + 1) * 64], in0=po[:, s4 * 65:s4 * 65 + 64], scalar1=fac[:, s4:s4 + 1], scalar2=None, op0=ALU.mult),
                         reads=[po, fac], writes=[oacc])
            else:
                c.dve.op(lambda e, po=po, s4=s4, h=h: e.scalar_tensor_tensor(out=oacc[:, s4, h * 64:(h + 1) * 64], in0=po[:, s4 * 65:s4 * 65 + 64], scalar=fac[:, s4:s4 + 1], op0=ALU.mult,
                                                                             in1=oacc[:, s4, h * 64:(h + 1) * 64], op1=ALU.add),
                         reads=[po, fac, oacc], writes=[oacc])

    def pv(po, pt, v_tk, v_ap, subs, started):
        for s4 in subs:
            st = not started[0]
            started[0] = True
            c.pe.op(lambda e, po=po, pt=pt, v_ap=v_ap, s4=s4, st=st: e.matmul(po[:, s4 * 65:(s4 + 1) * 65], lhsT=pt[:, s4 * 128:(s4 + 1) * 128], rhs=v_ap, start=st, stop=True, skip_group_check=True),
                    reads=[pt, v_tk], writes=[po])

    for qt in range(NT):
        t0 = qt * TT
        b = qt % 2
        tsl = slice(t0, t0 + TT)
        c.dma(c.sp, dQA[b], QA[b][0:64, :, :], QS[:, :, tsl], reads=[QS], writes=[QA[b]])
        c.dma(c.sp, dgt, gt[:], GS[tsl, :].rearrange("(s p) f -> p s f", p=128), reads=[GS], writes=[gt])
        lo = max(0, t0 - 512)
        jlo = (lo - (t0 - 512)) // 128
        c.dma(c.sp, dKW[b], KWt[b][0:64, :, jlo * 128:1024], KSs["kw"][:, :, lo:t0 + 512], reads=[KSs["kw"]], writes=[KWt[b]])
        c.dma(c.sp, dVW[b], [VWt[b][:, jlo:8, g, 0:64] for g in range(2)], [VSs["vw"][lo:t0 + 512, g, :].rearrange("(k p) d -> p k d", p=128) for g in range(2)], reads=[VSs["vw"]], writes=[VWt[b]])
        for g in range(2):
            pipe.depth = 2
            sbanks[0] = [P[0], P[1], P[2]]
            nmax = (t0 + 511 - 31) // 16
            nkc = nmax // 128 + 1
            for hh in range(8):
                h = g * 8 + hh
                po = P[3 + poi[0] % 2]
                pim = P[5 + poi[0] % 2]
                poi[0] += 1
                st_o = [False]
                st_i = [False]
                for ktc in range(nkc):
                    off = t0 - 2048 * ktc
                    extra = []
                    if 2048 + 15 > off:
                        v = off // 512
                        extra.append((identb[:], pmask[:, v, :], [identb, pmask]))
                    pt = score_tile((KC, KC[0:67, g, ktc * 128:(ktc + 1) * 128]), (QA[b], QA[b][0:67, h, :]), extra, bcol(("c", h, off - 31)))

                    def _cpv(po=po, pim=pim, pt=pt, ktc=ktc, g=g, st_o=st_o, st_i=st_i):
                        pv(po, pt, VCM, VCM[:, ktc, g, :], range(4), st_o)
                        for s4 in range(4):
                            st = not st_i[0]
                            st_i[0] = True
                            c.pe.op(lambda e, s4=s4, st=st: e.matmul(pim[:, s4 * 128:(s4 + 1) * 128], lhsT=pt[:, s4 * 128:(s4 + 1) * 128], rhs=mcs[:, ktc, :], start=st, stop=True, skip_group_check=True),
                                    reads=[pt, mcs], writes=[pim])
                    pipe.push(_cpv)

                def _cfin(po=po, pim=pim, h=h, hh=hh):
                    finish_head(po, h, 0, True)
                    for s4 in range(4):
                        if hh == 0:
                            c.dve.op(lambda e, s4=s4: e.tensor_scalar(out=impacc[:, s4, :], in0=pim[:, s4 * 128:(s4 + 1) * 128], scalar1=rs[:, s4:s4 + 1], scalar2=None, op0=ALU.mult),
                                     reads=[pim, rs], writes=[impacc])
                        else:
                            c.dve.op(lambda e, s4=s4: e.scalar_tensor_tensor(out=impacc[:, s4, :], in0=pim[:, s4 * 128:(s4 + 1) * 128], scalar=rs[:, s4:s4 + 1], op0=ALU.mult, in1=impacc[:, s4, :], op1=ALU.add),
                                     reads=[pim, rs, impacc], writes=[impacc])
                pipe.push(_cfin)
            pipe.flush()
            for s4 in range(4):
                c0 = (t0 + 128 * s4) // 64
                c.dve.op(lambda e, s4=s4, c0=c0: e.tensor_tensor(out=impm[:], in0=impacc[:, s4, :], in1=cb[:, 128 - c0:256 - c0], op=ALU.add), reads=[impacc, cb], writes=[impm])
                c.dve.op(lambda e: e.tensor_scalar(out=impm[:, 0:1], in0=impm[:, 0:1], scalar1=1e4, scalar2=None, op0=ALU.add), reads=[impm], writes=[impm])
                c.dve.op(lambda e: e.max(out=mx8[:, 0:8], in_=impm[:]), reads=[impm], writes=[mx8])
                c.dve.op(lambda e: e.match_replace(out=rep[:], in_to_replace=mx8[:, 0:8], in_values=impm[:], imm_value=-1e30), reads=[impm, mx8], writes=[rep])
                c.dve.op(lambda e: e.max(out=mx8[:, 8:16], in_=rep[:]), reads=[rep], writes=[mx8])
                c.dve.op(lambda e: e.tensor_scalar(out=thr[:], in0=mx8[:, 15:16], scalar1=-0.5, scalar2=None, op0=ALU.max), reads=[mx8], writes=[thr])
                c.dve.op(lambda e: e.tensor_scalar(out=selB[:], in0=impm[:], scalar1=thr[:, 0:1], scalar2=-BIG, op0=ALU.is_lt, op1=ALU.mult), reads=[impm, thr], writes=[selB])
                ptb = P[7].ap.bitcast(BF16)
                c.pe.op(lambda e, ptb=ptb: e.transpose(out=ptb[:, 0:128], in_=selB[:], identity=identb[:]), reads=[selB, identb], writes=[P[7]])
                c.act.op(lambda e, s4=s4, ptb=ptb: e.copy(out=selT[:, s4 * 128:(s4 + 1) * 128], in_=ptb[:, 0:128]), reads=[P[7]], writes=[selT])
            pipe.depth = 4
            sbanks[0] = [P[0], P[1], P[2], P[5], P[6]]
            for hh in range(8):
                h = g * 8 + hh
                po = P[3 + poi[0] % 2]
                poi[0] += 1
                st_o = [False]
                for kt in range(4 * qt + 4):
                    w = (2 * kt) // 64
                    pt_i = ((2 * kt) % 64) // 2
                    extra = [(ew[64 * w:64 * w + 64, pt_i, :], selT[64 * w:64 * w + 64, :], [ew, selT])]
                    jd = kt - 4 * qt
                    if jd >= 0:
                        extra.append((identb[:], cmask[:, jd, :], [identb, cmask]))
                    pt = score_tile((KS, KS[0:67, g, kt * 128:(kt + 1) * 128]), (QA[b], QA[b][0:67, h, :]), extra, bcol(("s", h, 4 * qt - kt)), cr=((jd * 128, TT) if jd > 0 else (0, TT)))
                    pipe.push(lambda po=po, pt=pt, kt=kt, g=g, jd=jd, st_o=st_o: pv(po, pt, VS, VS[:, kt, g, :], [s4 for s4 in range(4) if jd <= s4], st_o))
                pipe.push(lambda po=po, h=h: finish_head(po, h, 1, False))
            for hh in range(8):
                h = g * 8 + hh
                po = P[3 + poi[0] % 2]
                poi[0] += 1
                st_o = [False]
                for j in range(8):
                    kt = 4 * qt - 4 + j
                    if kt < 0:
                        continue
                    mk = wmask[:, j, :] if j < 4 else cmask[:, j - 4, :]
                    mtk = wmask if j < 4 else cmask
                    extra = [(identb[:], mk, [identb, mtk])]
                    pt = score_tile((KWt[b], KWt[b][0:67, g, j * 128:(j + 1) * 128]), (QA[b], QA[b][0:67, h, :]), extra, bcol(("s", h, 4 * qt - kt)), cr=((0, (j + 1) * 128) if j < 4 else ((j - 4) * 128, TT)))
                    subs = [s4 for s4 in range(4) if (s4 <= j if j < 4 else s4 >= j - 4)]
                    pipe.push(lambda po=po, pt=pt, j=j, g=g, b=b, subs=subs, st_o=st_o: pv(po, pt, VWt[b], VWt[b][:, j, g, :], subs, st_o))
                pipe.push(lambda po=po, h=h: finish_head(po, h, 2, False))
            pipe.flush()
        for s4 in range(4):
            for half in range(2):
                pb = P[(2 * s4 + half) % 3]
                for k in range(4):
                    cc = half * 4 + k
                    c.pe.op(lambda e, cc=cc, s4=s4, k=k, pb=pb: e.transpose(out=pb[:, k * 128:(k + 1) * 128], in_=oacc[:, s4, cc * 128:(cc + 1) * 128], identity=identf[:]),
                            reads=[oacc, identf], writes=[pb])
                c.act.op(lambda e, s4=s4, half=half, pb=pb: e.copy(out=oTn[:, half * 4:(half + 1) * 4, s4 * 128:(s4 + 1) * 128], in_=pb[:, :].rearrange("p (k i) -> p k i", k=4)),
                         reads=[pb], writes=[oTn])
        c.dma(c.sp, dOS, OS[:, :, tsl], oTn[:], reads=[oTn], writes=[OS])
    sc.close()
    pd.close()

    sc = Scope(c)
    ws = WStream(c, sc, 3, 2816)
    fb = ffn_bufs(sc)
    hT = sc.sb("hT", [128, 8, TT], F32)
    yT = sc.sb("yT", [128, 8, TT], F32)
    xt = sc.sb("xt", [128, 4, D], F32)
    oTl = sc.sb("oTl", [128, 8, TT], BF16)
    wout1 = load_resident(sc, "wout1", A["c_w_out"], D)
    dx = c.dsem("dxE")
    dhl = c.dsem("dhlE")
    dol = c.dsem("dolE")
    for t in range(NT):
        tsl = slice(t * TT, (t + 1) * TT)
        if stage != "D":
            ws.push(ffn_items(3))
        c.dma(c.sp, dhl, hT[:], HS[:, :, tsl], reads=[HS], writes=[hT])
        c.dma(c.sp, dol, oTl[:], OS[:, :, tsl], reads=[OS], writes=[oTl])
        for cc in range(8):
            pb = P[cc % 2]
            for k in range(8):
                c.pe.op(lambda e, cc=cc, k=k, pb=pb: e.matmul(pb[:, :], lhsT=wout1[:, k, cc * 128:(cc + 1) * 128], rhs=oTl[:, k, :], start=(k == 0), stop=(k == 7)),
                        reads=[wout1, oTl], writes=[pb])
            c.dve.op(lambda e, cc=cc, pb=pb: e.tensor_tensor(out=hT[:, cc, :], in0=pb[:, :], in1=hT[:, cc, :], op=ALU.add), reads=[pb, hT], writes=[hT])
        if stage == "D":
            store_out_tile(t, hT, xt, dx)
            continue
        ffn(ws, fb, hT, 5)
        rmsnorm_fm(fb[3], hT, 6, yT)
        store_out_tile(t, yT, xt, dx)
    sc.close()
    c.emit()
    return nc


def prep_inputs(inp, b, T):
    f = lambda a: np.ascontiguousarray(a, dtype=np.float32)
    m = {}
    m["x"] = f(inp["x"][b, :T])
    m["ffn_wg"] = f(np.stack([inp["ffn1_wg"][0], inp["ffn1_wg"][1], inp["ffn2_wg"][0], inp["ffn2_wg"][1]]))
    m["ffn_wu"] = f(np.stack([inp["ffn1_wu"][0], inp["ffn1_wu"][1], inp["ffn2_wu"][0], inp["ffn2_wu"][1]]))
    m["ffn_wd"] = f(np.stack([inp["ffn1_wd"][0], inp["ffn1_wd"][1], inp["ffn2_wd"][0], inp["ffn2_wd"][1]]))
    gl = [inp["norm_ffn1"][0], inp["norm_ffn1"][1], inp["norm_mix"][0], inp["norm_mix"][1], inp["norm_ffn2"][0], inp["norm_ffn2"][1], inp["final_norm"]]
    m["gam"] = f(np.concatenate([np.asarray(g).reshape(8, 128).T for g in gl], axis=1))
    m["a_w_in"] = f(inp["a_w_in"][0])
    w2a = np.zeros((33, 256), np.float32)
    w2a[0:16] = inp["a_gate_w2"][0]
    w2a[32] = inp["a_gate_b"][0]
    m["w2a"] = w2a
    m["gnorm"] = f(inp["a_gla_norm"][0].reshape(1, 128))
    m["poolw"] = f(inp["a_pool_w"][0])
    m["pscale"] = f(inp["a_pool_scale"][0].reshape(4, 128).T)
    m["a_w_out"] = f(inp["a_w_out"][0])
    m["c_w_in"] = f(inp["c_w_in"][0])
    m["pef"] = f(inp["c_cmp_pe"][0].reshape(16, 128).T)
    for k in ("cmpk_w1", "cmpk_w2", "cmpv_w1", "cmpv_w2"):
        m[k] = f(inp["c_" + k][0])
    m["c_w_out"] = f(inp["c_w_out"][0])
    return m


_CACHE = {}


def run(inputs, T, stage="full", ncores=4):
    inputs = {k: np.asarray(v) for k, v in inputs.items()}
    key = (T, stage)
    if key not in _CACHE:
        _CACHE[key] = build_nc(T, stage)
    nc = _CACHE[key]
    cst = host_consts(T)

    in_maps = []
    shared = None
    for b in range(ncores):
        m = prep_inputs(inputs, b, T)
        if shared is None:
            shared = {k: v for k, v in m.items() if k != "x"}
        else:
            for k in shared:
                m[k] = shared[k]
        for k, v in cst.items():
            m["c_" + k] = v
        m["biasT"] = bias_table(T)
        in_maps.append(m)
    res = run_bass_kernel_spmd(nc, in_maps, core_ids=list(range(ncores)))
    return np.stack([np.asarray(r["out"], dtype=np.float32) for r in res.results], axis=0)


def kernel(**inputs):
    return run(inputs, 8192, "full", 4)
```
